# Optimizing a Trainium2 kernel written in Bass

```python
import jax, jax.numpy as jnp
from jax import lax
import numpy as np

D_MODEL = 1024
BATCH = 8
SEQ = 4096
DEPTH = 1

N_META = 16
BLOCK = 128
HEAD_DIM = 64
ROPE_THETA = 10000.0
EPS = 1e-6
NEG_INF = -1e30
SA_HEADS = 8
SA_KV_HEADS = 1
TOPK_MAX = 256
IDX_HEADS = 4
IDX_DIM = 64
SW_HEADS = 8
SW_KV_HEADS = 2
WINDOW = 128
D_FF = 2816

IN_SIZES = (
    SA_HEADS * HEAD_DIM,
    SA_KV_HEADS * HEAD_DIM,
    SA_KV_HEADS * HEAD_DIM,
    IDX_HEADS * IDX_DIM,
    IDX_DIM,
    IDX_HEADS,
    SW_HEADS * HEAD_DIM,
    SW_KV_HEADS * HEAD_DIM,
    SW_KV_HEADS * HEAD_DIM,
    D_MODEL,
    D_MODEL,
)
IN_WIDTH = int(sum(IN_SIZES))
IN_SPLITS = tuple(int(v) for v in np.cumsum(IN_SIZES)[:-1])

kernel_name = "hybrid_dsa_swa_sink_macaron_meta"


def rmsnorm(x, g):
    xf = x.astype(jnp.float32)
    y = xf * lax.rsqrt(jnp.mean(xf * xf, axis=-1, keepdims=True) + EPS)
    return (y * g.astype(jnp.float32)).astype(x.dtype)


def rope(x, pos):
    d = x.shape[-1]
    inv_freq = 1.0 / (ROPE_THETA ** (jnp.arange(0, d, 2, dtype=jnp.float32) / d))
    ang = pos.astype(jnp.float32)[:, None] * inv_freq[None, :]
    cos = jnp.cos(ang)[None, :, None, :].astype(x.dtype)
    sin = jnp.sin(ang)[None, :, None, :].astype(x.dtype)
    x1, x2 = x[..., : d // 2], x[..., d // 2:]
    return jnp.concatenate([x1 * cos - x2 * sin, x2 * cos + x1 * sin], axis=-1)


def swiglu(x, w_gate, w_up, w_down):
    return (jax.nn.silu(x @ w_gate) * (x @ w_up)) @ w_down


def sparse_branch(qa, ka, va, qi, ki, wi, pos, kvalid, topk):
    B, N = qa.shape[0], qa.shape[1]
    NC = N // BLOCK
    scale = HEAD_DIM ** -0.5

    def to_blocks(t):
        return jnp.moveaxis(t.reshape((B, NC, BLOCK) + t.shape[2:]), 1, 0)

    def one_block(args):
        qa_b, qi_b, wi_b, qpos = args
        rel = jax.nn.relu(jnp.einsum('bqhd,bsd->bqhs', qi_b, ki).astype(jnp.float32))
        score = jnp.einsum('bqhs,bqh->bqs', rel, wi_b.astype(jnp.float32))
        admissible = kvalid[None, :] & (pos[None, :] <= qpos[:, None])
        score = jnp.where(admissible[None], score, NEG_INF)
        top_val, top_idx = lax.top_k(score, topk)
        sel_ok = top_val > 0.5 * NEG_INF
        k_sel = jax.vmap(lambda a, i: a[i])(ka, top_idx)
        v_sel = jax.vmap(lambda a, i: a[i])(va, top_idx)
        s = jnp.einsum('bqhd,bqkd->bqhk', qa_b, k_sel).astype(jnp.float32) * scale
        s = jnp.where(sel_ok[:, :, None, :], s, NEG_INF)
        p = jax.nn.softmax(s, axis=-1).astype(va.dtype)
        o = jnp.einsum('bqhk,bqkd->bqhd', p, v_sel)
        return o.reshape(B, BLOCK, SA_HEADS * HEAD_DIM)

    out = lax.map(one_block, (to_blocks(qa), to_blocks(qi), to_blocks(wi), pos.reshape(NC, BLOCK)))
    return jnp.moveaxis(out, 0, 1).reshape(B, N, SA_HEADS * HEAD_DIM)


def swa_branch(qs, ks, vs, sinks, pos, kvalid):
    B, N = qs.shape[0], qs.shape[1]
    NC = N // BLOCK
    G = SW_HEADS // SW_KV_HEADS
    scale = HEAD_DIM ** -0.5
    q = qs.reshape(B, NC, BLOCK, SW_KV_HEADS, G, HEAD_DIM)
    k = ks.reshape(B, NC, BLOCK, SW_KV_HEADS, HEAD_DIM)
    v = vs.reshape(B, NC, BLOCK, SW_KV_HEADS, HEAD_DIM)
    shift = lambda t: jnp.concatenate([jnp.zeros_like(t[:, :1]), t[:, :-1]], axis=1)
    kb = jnp.concatenate([shift(k), k], axis=2)
    vb = jnp.concatenate([shift(v), v], axis=2)
    pos_c = pos.reshape(NC, BLOCK)
    val_c = kvalid.reshape(NC, BLOCK)
    prev_pos = jnp.concatenate([jnp.full((1, BLOCK), -1, pos.dtype), pos_c[:-1]], axis=0)
    prev_val = jnp.concatenate([jnp.zeros((1, BLOCK), bool), val_c[:-1]], axis=0)
    kpos = jnp.concatenate([prev_pos, pos_c], axis=1)
    kval = jnp.concatenate([prev_val, val_c], axis=1)
    diff = pos_c[:, :, None] - kpos[:, None, :]
    mask = kval[:, None, :] & (diff >= 0) & (diff < WINDOW)
    s = jnp.einsum('bcqkgd,bcskd->bckgqs', q, kb).astype(jnp.float32) * scale
    s = jnp.where(mask[None, :, None, None], s, NEG_INF)
    sink = sinks.astype(jnp.float32).reshape(1, 1, SW_KV_HEADS, G, 1, 1)
    m = jnp.maximum(jnp.max(s, axis=-1, keepdims=True), sink)
    e = jnp.exp(s - m)
    p = e / (jnp.sum(e, axis=-1, keepdims=True) + jnp.exp(sink - m))
    o = jnp.einsum('bckgqs,bcskd->bcqkgd', p.astype(vs.dtype), vb)
    return o.reshape(B, N, SW_HEADS * HEAD_DIM)


def setup_inputs(seed: int = 0) -> dict:
    key = jax.random.key(seed)
    ks = jax.random.split(key, 20)
    f32 = jnp.float32
    nrm = lambda k, shape, fan_in: jax.random.normal(k, shape, f32) * (fan_in ** -0.5)
    gain = lambda k, shape: 1.0 + 0.01 * jax.random.normal(k, shape, f32)
    L = DEPTH
    return {
        "x": jax.random.normal(ks[0], (BATCH, SEQ, D_MODEL), f32),
        "meta_tokens": jax.random.normal(ks[1], (N_META, D_MODEL), f32),
        "norm_ffn1": gain(ks[2], (L, D_MODEL)),
        "w_ffn1_gate": nrm(ks[3], (L, D_MODEL, D_FF), D_MODEL),
        "w_ffn1_up": nrm(ks[4], (L, D_MODEL, D_FF), D_MODEL),
        "w_ffn1_down": nrm(ks[5], (L, D_FF, D_MODEL), D_FF),
        "norm_mix": gain(ks[6], (L, D_MODEL)),
        "w_in": nrm(ks[7], (L, D_MODEL, IN_WIDTH), D_MODEL),
        "sinks": 0.5 * jax.random.normal(ks[8], (L, SW_HEADS), f32),
        "w_branch_sparse": nrm(ks[9], (L, SA_HEADS * HEAD_DIM, D_MODEL), SA_HEADS * HEAD_DIM),
        "w_branch_swa": nrm(ks[10], (L, SW_HEADS * HEAD_DIM, D_MODEL), SW_HEADS * HEAD_DIM),
        "w_out": nrm(ks[11], (L, D_MODEL, D_MODEL), D_MODEL),
        "norm_ffn2": gain(ks[12], (L, D_MODEL)),
        "w_ffn2_gate": nrm(ks[13], (L, D_MODEL, D_FF), D_MODEL),
        "w_ffn2_up": nrm(ks[14], (L, D_MODEL, D_FF), D_MODEL),
        "w_ffn2_down": nrm(ks[15], (L, D_FF, D_MODEL), D_FF),
        "norm_final": gain(ks[16], (D_MODEL,)),
    }


def reference(x, meta_tokens, norm_ffn1, w_ffn1_gate, w_ffn1_up, w_ffn1_down, norm_mix, w_in, sinks,
              w_branch_sparse, w_branch_swa, w_out, norm_ffn2, w_ffn2_gate, w_ffn2_up, w_ffn2_down, norm_final):
    B, S, D = x.shape
    topk = min(TOPK_MAX, S // 4)
    n_pad = BLOCK - N_META
    h = jnp.concatenate([
        jnp.zeros((B, n_pad, D), x.dtype),
        jnp.broadcast_to(meta_tokens.astype(x.dtype)[None], (B, N_META, D)),
        x,
    ], axis=1)
    N = h.shape[1]
    pos = jnp.arange(N, dtype=jnp.int32) - n_pad
    kvalid = pos >= 0
    rpos = jnp.maximum(pos, 0)
    idx_scale = (IDX_HEADS ** -0.5) * (IDX_DIM ** -0.5)

    for l in range(DEPTH):
        h = h + 0.5 * swiglu(rmsnorm(h, norm_ffn1[l]), w_ffn1_gate[l], w_ffn1_up[l], w_ffn1_down[l])

        u = rmsnorm(h, norm_mix[l])
        qa, ka, va, qi, ki, wi, qs, ksw, vsw, ga, gb = jnp.split(u @ w_in[l], IN_SPLITS, axis=-1)
        qa = rope(qa.reshape(B, N, SA_HEADS, HEAD_DIM), rpos)
        ka = rope(ka.reshape(B, N, SA_KV_HEADS, HEAD_DIM), rpos)[:, :, 0]
        va = va.reshape(B, N, HEAD_DIM)
        qi = rope(qi.reshape(B, N, IDX_HEADS, IDX_DIM), rpos)
        ki = rope(ki.reshape(B, N, 1, IDX_DIM), rpos)[:, :, 0]
        wi = wi * idx_scale
        qs = rope(qs.reshape(B, N, SW_HEADS, HEAD_DIM), rpos)
        ksw = rope(ksw.reshape(B, N, SW_KV_HEADS, HEAD_DIM), rpos)
        vsw = vsw.reshape(B, N, SW_KV_HEADS, HEAD_DIM)

        o_sparse = sparse_branch(qa, ka, va, qi, ki, wi, pos, kvalid, topk)
        o_swa = swa_branch(qs, ksw, vsw, sinks[l], pos, kvalid)

        merged = (jax.nn.sigmoid(ga) * (o_sparse @ w_branch_sparse[l])
                  + jax.nn.sigmoid(gb) * (o_swa @ w_branch_swa[l]))
        h = h + merged @ w_out[l]

        h = h + 0.5 * swiglu(rmsnorm(h, norm_ffn2[l]), w_ffn2_gate[l], w_ffn2_up[l], w_ffn2_down[l])

    return rmsnorm(h[:, BLOCK:], norm_final)
```

```python
import numpy as np
from contextlib import ExitStack
import concourse.bass as bass
import concourse.mybir as mybir
from concourse.bass_utils import run_bass_kernel_spmd

F32 = mybir.dt.float32
BF16 = mybir.dt.bfloat16
AF = mybir.ActivationFunctionType
ALU = mybir.AluOpType
AX = mybir.AxisListType

D = 1024
SEQ = 4096
NBLK = 33
FF = 2816
NJ = 22
NSLOT = 34
NIT = 16
EPS = 1e-6
IDX_SCALE = (4 ** -0.5) * (64 ** -0.5)
NEGM = -30000.0


class TT:
    __slots__ = ("name", "w", "r")

    def __init__(self, name):
        self.name = name
        self.w = None
        self.r = {}


class Sched:
    ENGS = ("pe", "act", "dve", "pool", "sp")

    def __init__(self, nc, n_dma_sems=12):
        self.nc = nc
        self.ops = {e: [] for e in self.ENGS}
        self.cnt = {e: 0 for e in self.ENGS}
        self.seen = {e: {} for e in self.ENGS}
        self.n_dma = n_dma_sems
        self.dma_val = {}
        self.dma_rr = {"sp": 0, "pool": 0, "act": 0}
        self.sems = {}
        self.final_waits = []

    def _need(self, eng, deps, key, val):
        if self.seen[eng].get(key, 0) >= val:
            return
        deps[key] = max(deps.get(key, 0), val)

    def _deps(self, eng, reads, writes, is_dma=False):
        deps = {}
        for t in reads:
            if t.w is not None:
                self._need(eng, deps, t.w[0], t.w[1])
        for t in writes:
            if t.w is not None and (is_dma or t.w[0] != eng):
                self._need(eng, deps, t.w[0], t.w[1])
            for k, v in t.r.items():
                if is_dma or k != eng:
                    self._need(eng, deps, k, v)
        for k, v in deps.items():
            self.seen[eng][k] = v
        return list(deps.items())

    def op(self, eng, fn, reads=(), writes=()):
        waits = self._deps(eng, reads, writes)
        self.cnt[eng] += 1
        c = self.cnt[eng]
        self.ops[eng].append((waits, fn, (eng, 1)))
        for t in reads:
            t.r[eng] = c
        for t in writes:
            t.w = (eng, c)
            t.r = {}
        return c

    def dma(self, q, fn, reads=(), writes=()):
        s = self.dma_rr[q]
        self.dma_rr[q] = (s + 1) % self.n_dma
        key = "d%s%d" % (q, s)
        s = key
        waits = self._deps(q, reads, writes, is_dma=True)
        prev = self.dma_val.get(s, 0)
        if prev > 0 and self.seen[q].get(key, 0) < prev:
            waits.append((key, prev))
            self.seen[q][key] = prev
        self.dma_val[s] = prev + 16
        v = self.dma_val[s]
        self.ops[q].append((waits, fn, (key, 16)))
        for t in reads:
            t.r[key] = v
        for t in writes:
            t.w = (key, v)
            t.r = {}

    def finish(self, eng, tiles):
        waits = [t.w for t in tiles if t.w is not None]
        self.final_waits.append((eng, waits))

    def emit(self, stack):
        nc = self.nc
        keys = list(self.ENGS) + sorted(self.dma_val.keys())
        for k in keys:
            self.sems[k] = stack.enter_context(nc.semaphore("s_" + k))
        block = stack.enter_context(nc.Block())
        sems = self.sems

        def run(eng_name):
            def body(e):
                for waits, fn, inc in self.ops[eng_name]:
                    for k, v in waits:
                        e.wait_ge(sems[k], v)
                    ins = fn(e)
                    ins.then_inc(sems[inc[0]], inc[1])
                for en, waits in self.final_waits:
                    if en == eng_name:
                        for k, v in waits:
                            e.wait_ge(sems[k], v)
            return body

        block.tensor(run("pe"))
        block.scalar(run("act"))
        block.vector(run("dve"))
        block.gpsimd(run("pool"))
        block.sync(run("sp"))


def build_program(stage=99):
    nc = bass.Bass("TRN2", target_bir_lowering=False, dynamic_dma_scratch_size=2048)
    dt = nc.dram_tensor
    x_d = dt("x", [SEQ, D], F32, kind="ExternalInput").ap()
    meta_d = dt("meta", [16, D], F32, kind="ExternalInput").ap()
    ws_d = dt("ws", [NSLOT, 128, 4096], F32, kind="ExternalInput").ap()
    wd_d = dt("wd", [2, 128, NJ * 1024], F32, kind="ExternalInput").ap()
    gcol_d = dt("gcol", [128, 24], F32, kind="ExternalInput").ap()
    gfin_d = dt("gfin", [D], F32, kind="ExternalInput").ap()
    sink_d = dt("sink", [8], F32, kind="ExternalInput").ap()
    cs_d = dt("cs", [NBLK * 128, 64], F32, kind="ExternalInput").ap()
    cst_d = dt("cst", [128, 1152], F32, kind="ExternalInput").ap()
    p2_d = dt("p2", [128, 32], F32, kind="ExternalInput").ap()
    wsb_d = dt("wsb", [NSLOT, 128, 4096], BF16, kind="Internal").ap()
    wdb_d = dt("wdb", [2, 128, NJ * 1024], BF16, kind="Internal").ap()
    out_d = dt("out", [SEQ, D], F32, kind="ExternalOutput").ap()

    with ExitStack() as st:
        def sb(n, s, d):
            return st.enter_context(nc.sbuf_tensor(n, s, d))

        def ps(n, s, d):
            return st.enter_context(nc.psum_tensor(n, s, d))

        PA = ps("PA", [128, 4, 512], F32)
        PB = ps("PB", [128, 2, 512], F32)
        PTR = ps("PTR", [128, 2, 1024], BF16)

        X = sb("X", [128, 4, 1024], F32)
        XT = sb("XT", [128, 8, 512], BF16)
        BIG = sb("BIG", [128, 7168], F32)
        AT = BIG[:].bitcast(BF16)
        SG = sb("SG", [128, 2, 512], BF16)
        WR = sb("WR", [128, 3, 4096], BF16)
        WD = sb("WD", [128, NJ * 1024], BF16)
        SGA = sb("SGA", [128, 4, 1024], BF16)
        SGB = sb("SGB", [128, 4, 1024], BF16)
        ROT = sb("ROT", [128, 28, 64], BF16)
        QT = sb("QT", [128, 14, 128], BF16)
        KST = sb("KST", [128, 2, 2, 128], BF16)
        KAT = sb("KAT", [128, NBLK * 128], BF16)
        KIT = sb("KIT", [128, NBLK * 128], BF16)
        VA = sb("VA", [128, NBLK, 66], BF16)
        VS = sb("VS", [128, 5, 2, 66], BF16)
        SC = sb("SC", [128, NBLK * 128], F32)
        NEG = sb("NEG", [128, NBLK * 128], BF16)
        RL = sb("RL", [128, 4, 512], F32)
        PT = sb("PT", [128, 2, 1024], BF16)
        OSP = sb("OSP", [128, 512], BF16)
        OWP = sb("OWP", [128, 512], BF16)
        OT = sb("OT", [128, 8, 128], BF16)
        MG = sb("MG", [128, 1024], BF16)
        CAUSF = sb("CAUSF", [128, 128], F32)
        CB = sb("CB", [128, 1152], BF16)
        GFIN = sb("GFIN", [128, 1024], F32)
        CS = sb("CS", [128, 4, 64], F32)
        G = sb("G", [128, 24], F32)
        P2 = sb("P2", [128, 32], F32)
        SM = sb("SM", [128, 64], F32)
        WT = sb("WT", [128, 32], F32)
        WI = sb("WI", [128, 4, 4], F32)
        REC = sb("REC", [128, 8], F32)
        ESINK = sb("ESINK", [128, 8], F32)

        SS = SM[:, 0:4]
        MS = SM[:, 4:8]
        RSTD = SM[:, 8:12]
        MHALF = SM[:, 12:16]
        AMAX = SM[:, 16:17]
        LO = SM[:, 17:18]
        RNG = SM[:, 18:19]
        MID = SM[:, 19:20]
        CNT = SM[:, 20:21]
        TV = SM[:, 21:22]
        THR = SM[:, 22:23]
        DEN = SM[:, 24:32]
        SA = SM[:, 32:33]

        NEG1 = WD[:, 0:4224]
        QT1 = WD[:, 4224:6016].rearrange("p (g t) -> p g t", g=14)
        KSTW = WD[:, 6016:7040].rearrange("p (a k t) -> p a k t", a=4, k=2)
        QT2 = WD[:, 7040:8832].rearrange("p (g t) -> p g t", g=14)
        QTs = [QT, QT1, QT2]
        NEGs = [NEG, NEG1]
        S = Sched(nc)
        T = TT
        tX = [T("X%d" % i) for i in range(4)]
        tXT = T("XT")
        tATc = [T("AT%d" % j) for j in range(NJ)]
        tR = [T("R%d" % i) for i in range(4)]
        tSG = [T("SG0"), T("SG1")]
        tWR = [T("WR%d" % i) for i in range(3)]
        tWD = T("WD")
        tSGA = [T("SGA%d" % i) for i in range(4)]
        tSGB = [T("SGB%d" % i) for i in range(4)]
        tROT = T("ROT")
        tQTs = [T("QT0"), T("QT1"), T("QT2")]
        tNEGs = [T("NEG0"), T("NEG1")]
        tNEGAs = [T("NEGA0"), T("NEGA1")]
        tKSTW = [T("KSTW%d" % i) for i in range(4)]
        tKST0 = T("KST0")
        tKAT, tKIT, tVA = T("KAT"), T("KIT"), T("VA")
        tVS = [T("VS%d" % i) for i in range(5)]
        tSC = T("SC")
        tSA, tMID = T("SA"), T("MID")
        tRL = [T("RL%d" % i) for i in range(4)]
        tPT = [T("PT0"), T("PT1")]
        tOSP, tOWP, tOT, tMG = T("OSP"), T("OWP"), T("OT"), T("MG")
        tC = T("CONST")
        tCS = T("CS")
        tSM = T("SM")
        tBI = T("BI")
        tWI = T("WI")
        tREC = T("REC")
        tPA = [T("PA%d" % i) for i in range(4)]
        tPB = [T("PB0"), T("PB1")]
        tPTR = [T("PTR0"), T("PTR1")]
        tWSB = [T("wsb%d" % i) for i in range(NSLOT)]
        tWDB = [T("wdb0"), T("wdb1")]
        tOUT = [T("out%d" % i) for i in range(40)]
        tF = [T("F%d" % i) for i in range(4)]

        CF = RL[:].rearrange("p a b -> p (a b)")[:, 0:1152]
        S.dma("sp", lambda e: e.dma_start(out=CF, in_=cst_d), writes=[tC] + tRL)
        S.dma("sp", lambda e: e.dma_start(out=CAUSF[:], in_=cst_d[:, 512:640]), writes=[tC])
        S.dma("sp", lambda e: e.dma_start(out=G[:], in_=gcol_d), writes=[tC])
        S.dma("sp", lambda e: e.dma_start(out=P2[:], in_=p2_d), writes=[tC])
        S.dma("sp", lambda e: e.dma_start(out=GFIN[:], in_=gfin_d.partition_broadcast(128)), writes=[tC])
        S.dma("sp", lambda e: e.dma_start(out=ESINK[:], in_=sink_d.partition_broadcast(128)), writes=[tC])
        S.op("dve", lambda e: e.tensor_copy(out=CB[:], in_=CF), reads=[tC] + tRL, writes=[tC])
        S.op("act", lambda e: e.activation(out=ESINK[:], in_=ESINK[:], func=AF.Exp), reads=[tC], writes=[tC])
        S.op("pool", lambda e: e.memset(SM[:, 12:16], -0.5), writes=[tSM])
        S.op("pool", lambda e: e.memset(VA[:, :, 64:66], 1.0), writes=[tVA])
        S.op("pool", lambda e: e.memset(VS[:, :, :, 64:66], 1.0), writes=tVS)
        ID4 = CB[:, 0:512]
        CAUS = CAUSF[:]
        SWCUR = CB[:, 640:768]
        SWPREV = CB[:, 768:896]
        SWPREV1 = CB[:, 896:1024]

        def convert_group(conv_order, first_reads=()):
          fr = list(first_reads)
          for it in conv_order:
            if isinstance(it, str):
                f = int(it[2])
                for hlf in range(2):
                    sl = slice(hlf * 11 * 1024, (hlf + 1) * 11 * 1024)
                    S.dma("pool", lambda e, f=f, sl=sl: e.dma_start(out=wdb_d[f, :, sl], in_=wd_d[f, :, sl], max_dma_last_dim=8192), writes=[tWDB[f]])
            else:
                S.dma("pool", lambda e, it=it: e.dma_start(out=wsb_d[it], in_=ws_d[it], max_dma_last_dim=8192), reads=fr, writes=[tWSB[it]])
                fr = []

        tiles = [[0]] + [list(range(1 + 4 * t, 5 + 4 * t)) for t in range(8)]
        seq = []
        for ti, blks in enumerate(tiles):
            if ti == 0:
                seq += list(range(0, 15))
            else:
                seq += list(range(0, NSLOT))
        ring = {"issued": 0, "used": 0, "holds": set()}

        def issue_next():
            k = ring["issued"]
            if k >= len(seq):
                return
            pos = k % 3
            slot = seq[k]
            S.dma("sp", lambda e, pos=pos, slot=slot: e.dma_start(out=WR[:, pos, :], in_=wsb_d[slot]),
                  reads=[tWSB[slot]], writes=[tWR[pos]])
            ring["issued"] += 1

        def pump():
            k = ring["used"] - 1
            released = min([k] + list(ring["holds"]))
            while ring["issued"] < min(k + 3, released + 3, len(seq)):
                issue_next()

        def use_slot(expect, hold=False):
            k = ring["used"]
            assert seq[k] == expect, (k, seq[k], expect)
            ring["used"] += 1
            if hold:
                ring["holds"].add(k)
            pump()
            assert ring["issued"] > k
            return k % 3

        def unhold_all():
            ring["holds"].clear()
            pump()

        def load_wd(f, alias=False):
            wr = [tWD] + ((tQTs + tNEGs + tNEGAs + tKSTW) if alias else [])
            S.dma("pool", lambda e, f=f: e.dma_start(out=WD[:], in_=wdb_d[f]), reads=[tWDB[f]], writes=wr)

        SGf = SG[:].rearrange("p a b -> p (a b)")
        XSs = [PT[:, 0, :], PT[:, 1, :]]

        def norm_to_xt(NB, gi, ptr_par):
            for i in range(NB):
                S.op("act", lambda e, i=i: e.activation(out=SGf, in_=X[:, i, :], func=AF.Square, accum_out=SS[:, i:i + 1]),
                     reads=[tX[i]], writes=[tSG[0], tSG[1], tSM])
            S.op("dve", lambda e: e.tensor_scalar(out=MS[:, 0:NB], in0=SS[:, 0:NB], scalar1=1.0 / D, scalar2=EPS, op0=ALU.mult, op1=ALU.add),
                 reads=[tSM], writes=[tSM])
            S.op("pool", lambda e: e.tensor_tensor(out=RSTD[:, 0:NB], in0=MS[:, 0:NB], in1=MHALF[:, 0:NB], op=ALU.pow),
                 reads=[tSM], writes=[tSM])
            for i in range(NB):
                xs, txs = XSs[i % 2], tPT[i % 2]
                S.op("act", lambda e, i=i, xs=xs: e.mul(out=xs, in_=X[:, i, :], mul=RSTD[:, i:i + 1]), reads=[tX[i], tSM], writes=[txs])
                par = (ptr_par + i) % 2

                def tr(e, par=par, xs=xs):
                    ins = None
                    for kc in range(8):
                        ins = e.transpose(out=PTR[:, par, kc * 128:(kc + 1) * 128], in_=xs[:, kc * 128:(kc + 1) * 128], identity=CB[:, 0:128])
                    return ins
                S.op("pe", tr, reads=[txs, tC], writes=[tPTR[par]])
                S.op("dve", lambda e, i=i, par=par: e.tensor_tensor(
                    out=XT[:, :, i * 128:(i + 1) * 128],
                    in0=PTR[:, par, :].rearrange("p (k t) -> p k t", k=8),
                    in1=G[:, gi * 8:(gi + 1) * 8].unsqueeze(2).to_broadcast([128, 8, 128]), op=ALU.mult),
                    reads=[tPTR[par], tC], writes=[tXT])

        def ffn(f, NB, alias_tiles):
            TK = NB * 128
            base = 0 if f == 0 else 23
            first = True
            for j in range(NJ):
                if j % 2 == 0:
                    pos = use_slot(base + j // 2)
                cj = j % 2
                gp = j % 2
                woff = cj * 2048

                def gu(e, pos=pos, woff=woff, gp=gp):
                    ins = None
                    for g in range(2):
                        for kc in range(8):
                            o = woff + g * 1024 + kc * 128
                            ins = e.matmul(PA[:, g * 2 + gp, 0:TK], lhsT=WR[:, pos, o:o + 128], rhs=XT[:, kc, 0:TK], start=(kc == 0), stop=(kc == 7))
                    return ins
                S.op("pe", gu, reads=[tWR[pos], tXT], writes=[tPA[gp], tPA[2 + gp]])
                S.op("act", lambda e, gp=gp: e.activation(out=SG[:, gp, 0:TK], in_=PA[:, gp, 0:TK], func=AF.Silu), reads=[tPA[gp]], writes=[tSG[gp]])
                wr = [tATc[j]] + (alias_tiles if first else [])
                first = False
                S.op("dve", lambda e, j=j, gp=gp: e.tensor_tensor(out=AT[:, j * 512:j * 512 + TK], in0=PA[:, 2 + gp, 0:TK], in1=SG[:, gp, 0:TK], op=ALU.mult),
                     reads=[tPA[2 + gp], tSG[gp]], writes=wr)
            n = 0
            for i in range(NB):
                for half in range(2):
                    par = n % 2
                    n += 1

                    def dn(e, i=i, half=half, par=par):
                        ins = None
                        for j in range(NJ):
                            ins = e.matmul(PB[:, par, :], lhsT=AT[:, j * 512 + i * 128:j * 512 + (i + 1) * 128],
                                           rhs=WD[:, j * 1024 + half * 512:j * 1024 + (half + 1) * 512], start=(j == 0), stop=(j == NJ - 1))
                        return ins
                    S.op("pe", dn, reads=tATc + [tWD], writes=[tPB[par]])
                    S.op("dve", lambda e, i=i, half=half, par=par: e.scalar_tensor_tensor(
                        out=X[:, i, half * 512:(half + 1) * 512], in0=PB[:, par, :], scalar=0.5, in1=X[:, i, half * 512:(half + 1) * 512],
                        op0=ALU.mult, op1=ALU.add), reads=[tPB[par], tX[i]], writes=[tX[i]])

        Rv = BIG[:].rearrange("p (i c) -> p i c", i=4)

        def project(NB, blks, chunks):
            n = 0
            for cc in chunks:
                pos = use_slot(11 + cc)
                for i in range(NB):
                    par = n % 2
                    n += 1
                    c = blks[i]

                    def pj(e, pos=pos, i=i, par=par):
                        ins = None
                        for kc in range(8):
                            ins = e.matmul(PB[:, par, :], lhsT=XT[:, kc, i * 128:(i + 1) * 128], rhs=WR[:, pos, kc * 512:(kc + 1) * 512], start=(kc == 0), stop=(kc == 7))
                        return ins
                    S.op("pe", pj, reads=[tWR[pos], tXT], writes=[tPB[par]])
                    if cc < 3:
                        wr = [tR[i]] + (tATc if cc == 0 and i == 0 else [])
                        eng = "act" if (n % 2 == 0) else "dve"
                        if eng == "act":
                            S.op("act", lambda e, i=i, cc=cc, par=par: e.copy(out=Rv[:, i, cc * 512:(cc + 1) * 512], in_=PB[:, par, :]), reads=[tPB[par]], writes=wr)
                        else:
                            S.op("dve", lambda e, i=i, cc=cc, par=par: e.tensor_copy(out=Rv[:, i, cc * 512:(cc + 1) * 512], in_=PB[:, par, :]), reads=[tPB[par]], writes=wr)
                    elif cc == 3:
                        S.op("act", lambda e, i=i, par=par: e.copy(out=Rv[:, i, 1536:1792], in_=PB[:, par, 0:256]), reads=[tPB[par]], writes=[tR[i]])
                        S.op("act", lambda e, c=c, par=par: e.copy(out=VA[:, c, 0:64], in_=PB[:, par, 256:320]), reads=[tPB[par]], writes=[tVA])
                        S.op("act", lambda e, i=i, par=par: e.copy(out=VS[:, i + 1, :, 0:64], in_=PB[:, par, 320:448].rearrange("p (k d) -> p k d", k=2)),
                             reads=[tPB[par]], writes=[tVS[i + 1]])
                        S.op("act", lambda e, i=i, par=par: e.mul(out=WI[:, i, :], in_=PB[:, par, 448:452], mul=IDX_SCALE),
                             reads=[tPB[par]], writes=[tWI])
                    else:
                        gsel = (cc - 4) // 2
                        hh = (cc - 4) % 2
                        dst = SGA if gsel == 0 else SGB
                        tdst = tSGA if gsel == 0 else tSGB
                        S.op("act", lambda e, dst=dst, i=i, hh=hh, par=par: e.activation(out=dst[:, i, hh * 512:(hh + 1) * 512], in_=PB[:, par, :], func=AF.Sigmoid),
                             reads=[tPB[par]], writes=[tdst[i]])
                    yield

        T1 = RL[:, 0:2, :].rearrange("p a b -> p (a b)")[:, 0:896].rearrange("p (h d) -> p h d", h=28)
        T2 = RL[:, 2:4, :].rearrange("p a b -> p (a b)")[:, 0:896].rearrange("p (h d) -> p h d", h=28)

        def rope_block(i, c, tile0=False):
            QTx, tQ = QTs[i % 3], tQTs[i % 3]
            Rb = Rv[:, i, :].rearrange("p (h t d) -> p h t d", h=28, t=2)
            x1 = Rb[:, :, 0, :]
            x2 = Rb[:, :, 1, :]
            cos = CS[:, i, 0:32].unsqueeze(1).to_broadcast([128, 28, 32])
            sin = CS[:, i, 32:64].unsqueeze(1).to_broadcast([128, 28, 32])
            rl01, rl23 = [tRL[0], tRL[1]], [tRL[2], tRL[3]]
            P = "pool"
            S.op(P, lambda e: e.tensor_tensor(out=T1, in0=x1, in1=cos, op=ALU.mult), reads=[tR[i], tCS], writes=rl01)
            S.op(P, lambda e: e.tensor_tensor(out=T2, in0=x2, in1=sin, op=ALU.mult), reads=[tR[i], tCS], writes=rl23)
            S.op(P, lambda e: e.tensor_tensor(out=ROT[:, :, 0:32], in0=T1, in1=T2, op=ALU.subtract), reads=rl01 + rl23, writes=[tROT])
            S.op(P, lambda e: e.tensor_tensor(out=T1, in0=x2, in1=cos, op=ALU.mult), reads=[tR[i], tCS], writes=rl01)
            S.op(P, lambda e: e.tensor_tensor(out=T2, in0=x1, in1=sin, op=ALU.mult), reads=[tR[i], tCS], writes=rl23)
            S.op(P, lambda e: e.tensor_tensor(out=ROT[:, :, 32:64], in0=T1, in1=T2, op=ALU.add), reads=rl01 + rl23, writes=[tROT])
            ROTf = ROT[:].rearrange("p h d -> p (h d)")

            def tr(e):
                ins = None
                for g in range(14):
                    ins = e.transpose(out=PTR[:, g // 8, (g % 8) * 128:(g % 8 + 1) * 128], in_=ROTf[:, g * 128:(g + 1) * 128], identity=CB[:, 0:128])
                return ins
            S.op("pe", tr, reads=[tROT, tC], writes=[tPTR[0], tPTR[1]])
            S.op("act", lambda e: e.copy(out=QTx[:, 0:8, :], in_=PTR[:, 0, :].rearrange("p (g t) -> p g t", g=8)), reads=[tPTR[0]], writes=[tQ])
            S.op("dve", lambda e: e.tensor_copy(out=QTx[:, 8:14, :], in_=PTR[:, 1, 0:768].rearrange("p (g t) -> p g t", g=6)), reads=[tPTR[1]], writes=[tQ])
            S.op("pool", lambda e: e.tensor_copy(out=KAT[:, c * 128:(c + 1) * 128], in_=QTx[:, 6, :]), reads=[tQ], writes=[tKAT])
            S.op("pool", lambda e: e.tensor_copy(out=KIT[:, c * 128:(c + 1) * 128], in_=QTx[:, 7, :]), reads=[tQ], writes=[tKIT])
            if tile0 or i == 3:
                S.op("pool", lambda e: e.tensor_copy(out=KST[:, 0, :, :], in_=QTx[:, 12:14, :]), reads=[tQ], writes=[tKST0])
            if not tile0:
                S.op("pool", lambda e: e.tensor_copy(out=KSTW[:, i, :, :], in_=QTx[:, 12:14, :]), reads=[tQ], writes=[tKSTW[i]])

        def attn_core(keyblocks, out_sb, t_out, sink, QTx, tQ):
            nk = len(keyblocks)

            def emit_sc(jj):
                s = jj % 2
                kfn, negap, vfn, rds, swa = keyblocks[jj]

                def sc(e):
                    ins = None
                    for hf in range(2):
                        if not swa:
                            e.matmul(PA[:, 2 * s + hf, :], lhsT=kfn(hf, 0), rhs=QTx[hf * 64:(hf + 1) * 64, 0:4, :], start=True, stop=False)
                        else:
                            for kap in range(2):
                                e.matmul(PA[:, 2 * s + hf, kap * 256:(kap + 1) * 256], lhsT=kfn(hf, kap),
                                         rhs=QTx[hf * 64:(hf + 1) * 64, 8 + 2 * kap:10 + 2 * kap, :], start=(kap == 0), stop=False, skip_group_check=True)
                    for hf in range(2):
                        ins = e.matmul(PA[:, 2 * s + hf, :], lhsT=negap, rhs=ID4, start=False, stop=True, skip_group_check=True)
                    return ins
                S.op("pe", sc, reads=rds + [tQ, tC], writes=[tPA[2 * s], tPA[2 * s + 1]])

            def emit_exp_pv(jj):
                s = jj % 2
                kfn, negap, vfn, rds, swa = keyblocks[jj]
                S.op("act", lambda e: e.activation(out=PT[:, s, :], in_=PA[:, 2 * s:2 * s + 2, :].rearrange("p a b -> p (a b)"), func=AF.Exp, scale=0.125),
                     reads=[tPA[2 * s], tPA[2 * s + 1]], writes=[tPT[s]])

                def pv(e):
                    ins = None
                    for hf in range(2):
                        for pr in range(4):
                            ins = e.matmul(PB[:, hf, pr * 65:(pr + 1) * 65], lhsT=PT[:, s, (hf * 4 + pr) * 128:(hf * 4 + pr + 1) * 128], rhs=vfn(pr),
                                           start=(jj == 0 and pr == 0), stop=(jj == nk - 1 and pr == 3), skip_group_check=True)
                    return ins
                S.op("pe", pv, reads=rds + [tPT[s]], writes=[tPB[0], tPB[1]])

            emit_sc(0)
            for jj in range(nk):
                if jj + 1 < nk:
                    emit_sc(jj + 1)
                emit_exp_pv(jj)
                yield
            PBv = PB[:, :, 0:260].rearrange("p b (r e) -> p b r e", e=65)
            RECv = REC[:].rearrange("p (b r o) -> p b r o", b=2, o=1)
            if sink:
                S.op("dve", lambda e: e.tensor_tensor(out=RECv, in0=PBv[:, :, :, 64:65], in1=ESINK[:].rearrange("p (b r o) -> p b r o", b=2, o=1), op=ALU.add),
                     reads=[tPB[0], tPB[1], tC], writes=[tREC])
                S.op("dve", lambda e: e.reciprocal(out=REC[:], in_=REC[:]), reads=[tREC], writes=[tREC])
            else:
                S.op("dve", lambda e: e.reciprocal(out=RECv, in_=PBv[:, :, :, 64:65]), reads=[tPB[0], tPB[1]], writes=[tREC])
            S.op("dve", lambda e: e.tensor_tensor(out=out_sb[:].rearrange("p (b r d) -> p b r d", b=2, r=4), in0=PBv[:, :, :, 0:64],
                                                  in1=RECv.to_broadcast([128, 2, 4, 64]), op=ALU.mult),
                 reads=[tPB[0], tPB[1], tREC], writes=[t_out])

        def index_bisect(i, c, frac=0.42):
            split = frac < 1.0
            QTx, tQ = QTs[i % 3], tQTs[i % 3]
            NEGx, tNEG, tNEGA = NEGs[i % 2], tNEGs[i % 2], tNEGAs[i % 2]
            n = (c + 1) * 128
            nch = (n + 511) // 512
            for ch in range(nch):
                w = min(512, n - ch * 512)

                for pr in range(2):
                    def ix(e, ch=ch, w=w, pr=pr):
                        ins = None
                        for hf in range(2):
                            ins = e.matmul(PA[:, 2 * pr + hf, 0:w], lhsT=QTx[hf * 64:(hf + 1) * 64, 4 + pr, :],
                                           rhs=KIT[hf * 64:(hf + 1) * 64, ch * 512:ch * 512 + w], start=True, stop=True)
                        return ins
                    S.op("pe", ix, reads=[tQ, tKIT], writes=[tPA[2 * pr], tPA[2 * pr + 1]])
                for h in range(4):
                    S.op("act", lambda e, h=h, w=w: e.activation(out=RL[:, h, 0:w], in_=PA[:, h, 0:w], func=AF.Relu), reads=[tPA[h]], writes=[tRL[h]])
                S.op("dve", lambda e, ch=ch, w=w: e.tensor_scalar(out=SC[:, ch * 512:ch * 512 + w], in0=RL[:, 0, 0:w], scalar1=WI[:, i, 0:1], scalar2=None, op0=ALU.mult),
                     reads=[tRL[0], tWI], writes=[tSC])
                for h in range(1, 4):
                    S.op("dve", lambda e, ch=ch, w=w, h=h: e.scalar_tensor_tensor(out=SC[:, ch * 512:ch * 512 + w], in0=RL[:, h, 0:w], scalar=WI[:, i, h:h + 1],
                                                                                 in1=SC[:, ch * 512:ch * 512 + w], op0=ALU.mult, op1=ALU.add),
                         reads=[tRL[h], tWI, tSC], writes=[tSC])
            V = "dve"
            S.op(V, lambda e: e.tensor_reduce(out=AMAX, in_=SC[:, 0:n], axis=AX.X, op=ALU.max, apply_absolute_value=True), reads=[tSC], writes=[tBI])
            S.op(V, lambda e: e.memset(SC[:, 0:112], -1e30), reads=[tSC], writes=[tSC])
            S.op(V, lambda e: e.tensor_tensor(out=SC[:, c * 128:(c + 1) * 128], in0=SC[:, c * 128:(c + 1) * 128], in1=CAUS, op=ALU.add), reads=[tSC, tC], writes=[tSC])
            S.op(V, lambda e: e.tensor_scalar(out=LO, in0=AMAX, scalar1=-1.001, scalar2=-1e-20, op0=ALU.mult, op1=ALU.add), reads=[tBI], writes=[tBI])
            S.op(V, lambda e: e.tensor_tensor(out=RNG, in0=AMAX, in1=LO, op=ALU.subtract), reads=[tBI], writes=[tBI])
            S.op(V, lambda e: e.tensor_scalar(out=WT[:, 0:NIT + 1], in0=P2[:, 0:NIT + 1], scalar1=RNG, scalar2=None, op0=ALU.mult), reads=[tBI, tC], writes=[tBI])
            S.op(V, lambda e: e.tensor_tensor(out=MID, in0=LO, in1=WT[:, 0:1], op=ALU.add), reads=[tBI], writes=[tMID])
            nd = (min(c, max(1, int(frac * (c + 1) + 0.5))) * 128) if split else n
            na = n - nd
            for it in range(NIT):
                if split:
                    S.op("act", lambda e: e.activation(out=NEGx[:, nd:n], in_=SC[:, nd:n], func=AF.Sign, bias=MID, scale=-1.0, accum_out=SA),
                         reads=[tSC, tMID], writes=[tNEGA, tSA])
                S.op(V, lambda e: e.tensor_scalar(out=NEGx[:, 0:nd], in0=SC[:, 0:nd], scalar1=MID, scalar2=None, op0=ALU.is_gt, op1=ALU.add, accum_out=CNT),
                     reads=[tSC, tMID], writes=[tNEG, tBI])
                if split:
                    S.op(V, lambda e: e.scalar_tensor_tensor(out=CNT, in0=SA, scalar=-0.5, in1=CNT, op0=ALU.mult, op1=ALU.add), reads=[tSA, tBI], writes=[tBI])
                S.op(V, lambda e: e.tensor_scalar(out=TV, in0=CNT, scalar1=255.5 - na / 2.0, scalar2=0.5, op0=ALU.is_gt, op1=ALU.subtract), reads=[tBI], writes=[tBI])
                S.op(V, lambda e, it=it: e.scalar_tensor_tensor(out=MID, in0=TV, scalar=WT[:, it:it + 1], in1=MID, op0=ALU.mult, op1=ALU.add), reads=[tBI, tMID], writes=[tMID])
                yield
            S.op(V, lambda e: e.tensor_tensor(out=THR, in0=MID, in1=WT[:, NIT:NIT + 1], op=ALU.subtract), reads=[tBI, tMID], writes=[tBI])
            S.op(V, lambda e: e.tensor_scalar(out=NEGx[:, 0:n], in0=SC[:, 0:n], scalar1=THR, scalar2=NEGM, op0=ALU.is_le, op1=ALU.mult), reads=[tSC, tBI], writes=[tNEG, tNEGA])

        def attn_tail(i, c, pos_bs, pos_bw):
            QTx, tQ = QTs[i % 3], tQTs[i % 3]
            NEGx, tNEG, tNEGA = NEGs[i % 2], tNEGs[i % 2], tNEGAs[i % 2]
            kbs = []
            for j in range(c + 1):
                kbs.append((lambda hf, kap, j=j: KAT[hf * 64:(hf + 1) * 64, j * 128:(j + 1) * 128], NEGx[:, j * 128:(j + 1) * 128],
                            lambda pr, j=j: VA[:, j, 0:65], [tKAT, tNEG, tNEGA, tVA], False))
            yield from attn_core(kbs, OSP, tOSP, False, QTx, tQ)
            if i == 0:
                kprev = lambda hf, kap: KST[hf * 64:(hf + 1) * 64, 0, kap, :]
                tkp = tKST0
            else:
                kprev = lambda hf, kap: KSTW[hf * 64:(hf + 1) * 64, i - 1, kap, :]
                tkp = tKSTW[i - 1]
            kbs = [
                (kprev, SWPREV1 if c == 1 else SWPREV, lambda pr: VS[:, i, pr // 2, 0:65], [tkp, tVS[i], tC], True),
                (lambda hf, kap: KSTW[hf * 64:(hf + 1) * 64, i, kap, :], SWCUR, lambda pr: VS[:, i + 1, pr // 2, 0:65], [tKSTW[i], tVS[i + 1], tC], True),
            ]
            yield from attn_core(kbs, OWP, tOWP, True, QTx, tQ)
            par = i % 2

            def tro(e):
                ins = None
                for k in range(4):
                    e.transpose(out=PTR[:, par, k * 128:(k + 1) * 128], in_=OSP[:, k * 128:(k + 1) * 128], identity=CB[:, 0:128])
                for k in range(4):
                    ins = e.transpose(out=PTR[:, par, (4 + k) * 128:(5 + k) * 128], in_=OWP[:, k * 128:(k + 1) * 128], identity=CB[:, 0:128])
                return ins
            S.op("pe", tro, reads=[tOSP, tOWP, tC], writes=[tPTR[par]])
            S.op("act", lambda e: e.copy(out=OT[:].rearrange("p k t -> p (k t)"), in_=PTR[:, par, :]), reads=[tPTR[par]], writes=[tOT])

            def br(e):
                ins = None
                for b, pos in enumerate((pos_bs, pos_bw)):
                    for half in range(2):
                        for kc in range(4):
                            ins = e.matmul(PA[:, 2 * b + half, :], lhsT=OT[:, 4 * b + kc, :], rhs=WR[:, pos, kc * 1024 + half * 512:kc * 1024 + (half + 1) * 512],
                                           start=(kc == 0), stop=(kc == 3))
                return ins
            S.op("pe", br, reads=[tOT, tWR[pos_bs], tWR[pos_bw]], writes=tPA)
            M1 = RL[:, 0:2, :]
            M2 = RL[:, 2:4, :]
            S.op("dve", lambda e: e.tensor_tensor(out=M1, in0=PA[:, 0:2, :], in1=SGA[:, i, :].rearrange("p (a b) -> p a b", a=2), op=ALU.mult),
                 reads=[tPA[0], tPA[1], tSGA[i]], writes=[tRL[0], tRL[1]])
            S.op("dve", lambda e: e.tensor_tensor(out=M2, in0=PA[:, 2:4, :], in1=SGB[:, i, :].rearrange("p (a b) -> p a b", a=2), op=ALU.mult),
                 reads=[tPA[2], tPA[3], tSGB[i]], writes=[tRL[2], tRL[3]])
            S.op("pool", lambda e: e.tensor_tensor(out=MG[:].rearrange("p (a b) -> p a b", a=2), in0=M1, in1=M2, op=ALU.add), reads=tRL, writes=[tMG])
            par2 = (i + 1) % 2

            def trm(e):
                ins = None
                for k in range(8):
                    ins = e.transpose(out=PTR[:, par2, k * 128:(k + 1) * 128], in_=MG[:, k * 128:(k + 1) * 128], identity=CB[:, 0:128])
                return ins
            S.op("pe", trm, reads=[tMG, tC], writes=[tPTR[par2]])
            S.op("act", lambda e: e.copy(out=XT[:, :, i * 128:(i + 1) * 128], in_=PTR[:, par2, :].rearrange("p (k t) -> p k t", k=8)), reads=[tPTR[par2]], writes=[tXT])

        def out_proj(NB, pos_a, pos_b):
            n = 0
            for i in range(NB):
                for half in range(2):
                    par = n % 2
                    n += 1

                    def wo(e, i=i, half=half, par=par):
                        ins = None
                        for kc in range(8):
                            pos = pos_a if kc < 4 else pos_b
                            o = (kc % 4) * 1024 + half * 512
                            ins = e.matmul(PB[:, par, :], lhsT=XT[:, kc, i * 128:(i + 1) * 128], rhs=WR[:, pos, o:o + 512], start=(kc == 0), stop=(kc == 7))
                        return ins
                    S.op("pe", wo, reads=[tXT, tWR[pos_a], tWR[pos_b]], writes=[tPB[par]])
                    S.op("dve", lambda e, i=i, half=half, par=par: e.tensor_tensor(out=X[:, i, half * 512:(half + 1) * 512], in0=PB[:, par, :],
                                                                                   in1=X[:, i, half * 512:(half + 1) * 512], op=ALU.add),
                         reads=[tPB[par], tX[i]], writes=[tX[i]])

        def final_norm_store(NB, blks, ti):
            for i in range(NB):
                tf = tF[i]
                S.op("act", lambda e, i=i: e.activation(out=SGf, in_=X[:, i, :], func=AF.Square, accum_out=SM[:, 40 + i:41 + i]), reads=[tX[i]], writes=[tSG[0], tSG[1], tf])
                S.op("dve", lambda e, i=i: e.tensor_scalar(out=SM[:, 44 + i:45 + i], in0=SM[:, 40 + i:41 + i], scalar1=1.0 / D, scalar2=EPS, op0=ALU.mult, op1=ALU.add), reads=[tf], writes=[tf])
                S.op("pool", lambda e, i=i: e.tensor_tensor(out=SM[:, 48 + i:49 + i], in0=SM[:, 44 + i:45 + i], in1=MHALF[:, 0:1], op=ALU.pow), reads=[tf, tSM], writes=[tf])
                S.op("dve", lambda e, i=i: e.scalar_tensor_tensor(out=X[:, i, :], in0=X[:, i, :], scalar=SM[:, 48 + i:49 + i], in1=GFIN[:], op0=ALU.mult, op1=ALU.mult),
                     reads=[tX[i], tf, tC], writes=[tX[i]])
                r0 = (blks[i] - 1) * 128
                S.dma("pool", lambda e, i=i, r0=r0: e.dma_start(out=out_d[r0:r0 + 128, :], in_=X[:, i, :]), reads=[tX[i]], writes=[tOUT[ti * 4 + i]])
                if ti < 8:
                    r1 = r0 + 512
                    S.dma("pool", lambda e, i=i, r1=r1: e.dma_start(out=X[:, i, :], in_=x_d[r1:r1 + 128, :]), writes=[tX[i]])

        for ti, blks in enumerate(tiles):
            NB = len(blks)
            if ti == 0:
                S.op("pool", lambda e: e.memset(X[:, 0, :], 0.0), writes=[tX[0]])
                S.dma("pool", lambda e: e.dma_start(out=X[112:128, 0, :], in_=meta_d), writes=[tX[0]])
            elif ti == 1:
                r0 = (blks[0] - 1) * 128
                S.dma("pool", lambda e, r0=r0: e.dma_start(out=X[:, 0:4, :], in_=x_d[r0:r0 + 512, :].rearrange("(i p) d -> p i d", p=128)), writes=tX)
            c0 = blks[0] * 128
            S.dma("pool", lambda e, c0=c0, NB=NB: e.dma_start(out=CS[:, 0:NB, :], in_=cs_d[c0:c0 + NB * 128, :].rearrange("(i p) d -> p i d", p=128)), writes=[tCS])
            if ti == 0:
                convert_group(list(range(0, 11)) + ["wd0"] + list(range(11, 15)))
                load_wd(0)
            norm_to_xt(NB, 0, 0)
            if ti == 1:
                convert_group(list(range(15, NSLOT)) + ["wd1"], first_reads=[tXT])
            ffn(0, NB, tR)
            norm_to_xt(NB, 1, 0)
            for _ in project(NB, blks, range(0, 4)):
                pass
            if ti == 0:
                S.op("pool", lambda e: e.tensor_copy(out=VS[:, 0, :, 0:64], in_=VS[:, 1, :, 0:64]), reads=[tVS[1]], writes=[tVS[0]])
                rope_block(0, 0, tile0=True)
                continue
            rope_block(0, blks[0])
            gb0 = index_bisect(0, blks[0], frac=0.42)
            next(gb0)
            gp = project(NB, blks, range(4, 8))
            p_done = b_done = False
            while not (p_done and b_done):
                if not p_done:
                    try:
                        next(gp)
                    except StopIteration:
                        p_done = True
                if not b_done:
                    try:
                        next(gb0)
                    except StopIteration:
                        b_done = True
            pos_bs = use_slot(19, hold=True)
            pos_bw = use_slot(20, hold=True)
            rope_block(1, blks[1])
            for i in range(NB):
                ga = attn_tail(i, blks[i], pos_bs, pos_bw)
                na_steps = blks[i] + 3
                roped = (i + 2 >= NB)
                if i + 1 < NB:
                    gb = index_bisect(i + 1, blks[i + 1], frac=0.5)
                    next(gb)
                    ca, cb, a_done, b_done = 0, 1, False, False
                    while not (a_done and b_done):
                        if b_done or (not a_done and ca * NIT <= cb * na_steps):
                            try:
                                next(ga)
                                ca += 1
                            except StopIteration:
                                a_done = True
                            if not roped and ca >= na_steps // 2:
                                rope_block(i + 2, blks[i + 2])
                                roped = True
                        else:
                            try:
                                next(gb)
                                cb += 1
                            except StopIteration:
                                b_done = True
                    if not roped:
                        rope_block(i + 2, blks[i + 2])
                else:
                    for _ in ga:
                        pass
            load_wd(1, alias=True)
            S.op("pool", lambda e: e.tensor_copy(out=VS[:, 0, :, 0:64], in_=VS[:, 4, :, 0:64]), reads=[tVS[4]], writes=[tVS[0]])
            unhold_all()
            pos_a = use_slot(21, hold=True)
            pos_b = use_slot(22, hold=True)
            out_proj(NB, pos_a, pos_b)
            unhold_all()
            norm_to_xt(NB, 2, 0)
            ffn(1, NB, tR)
            if ti < 8:
                load_wd(0)
            final_norm_store(NB, blks, ti)
            if stage == 5 and ti == 1:
                break
        S.finish("pool", tOUT)
        S.emit(st)
    return nc


def _host_layout(inp):
    f32 = np.float32
    w_in = np.asarray(inp["w_in"][0], f32)
    cols = np.arange(3780)
    qa, ka, va = cols[0:512], cols[512:576], cols[576:640]
    qi, ki, wi = cols[640:896], cols[896:960], cols[960:964]
    qs, ksw, vsw = cols[964:1476], cols[1476:1604], cols[1604:1732]
    ga, gb = cols[1732:2756], cols[2756:3780]
    perm = np.concatenate([qa, qi, ka, ka, ki, ki, qs, ksw[:64], ksw[:64], ksw[64:], ksw[64:], va, vsw, wi])
    win_p = np.zeros((D, 4096), f32)
    win_p[:, :perm.size] = w_in[:, perm]
    win_p[:, 2048:3072] = w_in[:, ga]
    win_p[:, 3072:4096] = w_in[:, gb]

    ws = np.zeros((NSLOT, 128, 4096), f32)

    def gu_slots(wg, wu, base):
        wg = np.asarray(wg, f32).reshape(8, 128, NJ, 128)
        wu = np.asarray(wu, f32).reshape(8, 128, NJ, 128)
        both = np.stack([wg, wu], 0)
        both = both.reshape(2, 8, 128, 11, 2, 128)
        arr = both.transpose(3, 2, 4, 0, 1, 5)
        ws[base:base + 11] = arr.reshape(11, 128, 4096)

    gu_slots(inp["w_ffn1_gate"][0], inp["w_ffn1_up"][0], 0)
    gu_slots(inp["w_ffn2_gate"][0], inp["w_ffn2_up"][0], 23)
    wp = win_p.reshape(8, 128, 8, 512)
    ws[11:19] = wp.transpose(2, 1, 0, 3).reshape(8, 128, 4096)
    heads = [2 * (sl % 4) + sl // 4 for sl in range(8)]
    rperm = np.concatenate([np.arange(h * 64, (h + 1) * 64) for h in heads])
    for k, name in enumerate(("w_branch_sparse", "w_branch_swa")):
        wb = np.asarray(inp[name][0], f32)[rperm]
        ws[19 + k] = wb.reshape(4, 128, 1024).transpose(1, 0, 2).reshape(128, 4096)
    wo = np.asarray(inp["w_out"][0], f32).reshape(8, 128, 1024)
    ws[21] = wo[0:4].transpose(1, 0, 2).reshape(128, 4096)
    ws[22] = wo[4:8].transpose(1, 0, 2).reshape(128, 4096)

    wd = np.stack([np.asarray(inp["w_ffn1_down"][0], f32).reshape(NJ, 128, 1024).transpose(1, 0, 2).reshape(128, NJ * 1024),
                   np.asarray(inp["w_ffn2_down"][0], f32).reshape(NJ, 128, 1024).transpose(1, 0, 2).reshape(128, NJ * 1024)], 0)

    gcol = np.concatenate([np.asarray(inp[k][0], f32).reshape(8, 128).T for k in ("norm_ffn1", "norm_mix", "norm_ffn2")], 1)
    sink = np.asarray(inp["sinks"][0], f32)[heads]

    pos = np.maximum(np.arange(NBLK * 128, dtype=np.int32) - 112, 0).astype(f32)
    inv_freq = (1.0 / (np.float32(10000.0) ** (np.arange(0, 64, 2, dtype=f32) / np.float32(64)))).astype(f32)
    ang = (pos[:, None] * inv_freq[None, :]).astype(f32)
    cs = np.concatenate([np.cos(ang).astype(f32), np.sin(ang).astype(f32)], 1)

    q = np.arange(128)[:, None]
    s = np.arange(128)[None, :]
    cst = np.zeros((128, 1152), f32)
    cst[:, 0:512] = np.tile(np.eye(128, dtype=f32), (1, 4))
    cst[:, 512:640] = np.where(s <= q, 0.0, -1e30)
    cst[:, 640:768] = np.where(s <= q, 0.0, NEGM)
    cst[:, 768:896] = np.where(s > q, 0.0, NEGM)
    cst[:, 896:1024] = np.where((s > q) & (s >= 112), 0.0, NEGM)
    p2 = np.tile((2.0 ** -(np.arange(32, dtype=np.float64) + 1)).astype(f32)[None, :], (128, 1))
    shared = {"meta": np.ascontiguousarray(np.asarray(inp["meta_tokens"], f32)), "ws": ws, "wd": np.ascontiguousarray(wd),
              "gcol": np.ascontiguousarray(gcol), "gfin": np.asarray(inp["norm_final"], f32), "sink": np.ascontiguousarray(sink),
              "cs": cs, "cst": cst, "p2": p2}
    return shared


_NC_CACHE = {}


def kernel(**inputs):
    shared = _host_layout(inputs)
    x = np.asarray(inputs["x"], np.float32)
    if "nc" not in _NC_CACHE:
        _NC_CACHE["nc"] = build_program()
    nc = _NC_CACHE["nc"]
    in_maps = []
    for b in range(8):
        m = dict(shared)
        m["x"] = np.ascontiguousarray(x[b])
        in_maps.append(m)
    res = run_bass_kernel_spmd(nc, in_maps, core_ids=list(range(8)))
    return np.stack([np.asarray(r["out"], np.float32) for r in res.results], 0)
```

```python
import numpy as np
from contextlib import ExitStack
import concourse.bass as bass
import concourse.mybir as mybir
from concourse.bass_utils import run_bass_kernel_spmd

F32 = mybir.dt.float32
BF16 = mybir.dt.bfloat16
AF = mybir.ActivationFunctionType
ALU = mybir.AluOpType
AX = mybir.AxisListType

D = 1024
SEQ = 4096
NBLK = 33
FF = 2816
NJ = 22
NSLOT = 34
NIT = 16
EPS = 1e-6
IDX_SCALE = (4 ** -0.5) * (64 ** -0.5)
NEGM = -30000.0


class TT:
    __slots__ = ("name", "w", "r")

    def __init__(self, name):
        self.name = name
        self.w = None
        self.r = {}


class Sched:
    ENGS = ("pe", "act", "dve", "pool", "sp")

    def __init__(self, nc, n_dma_sems=12):
        self.nc = nc
        self.ops = {e: [] for e in self.ENGS}
        self.cnt = {e: 0 for e in self.ENGS}
        self.seen = {e: {} for e in self.ENGS}
        self.n_dma = n_dma_sems
        self.dma_val = {}
        self.dma_rr = {"sp": 0, "pool": 0, "act": 0}
        self.sems = {}
        self.final_waits = []

    def _need(self, eng, deps, key, val):
        if self.seen[eng].get(key, 0) >= val:
            return
        deps[key] = max(deps.get(key, 0), val)

    def _deps(self, eng, reads, writes, is_dma=False):
        deps = {}
        for t in reads:
            if t.w is not None:
                self._need(eng, deps, t.w[0], t.w[1])
        for t in writes:
            if t.w is not None and (is_dma or t.w[0] != eng):
                self._need(eng, deps, t.w[0], t.w[1])
            for k, v in t.r.items():
                if is_dma or k != eng:
                    self._need(eng, deps, k, v)
        for k, v in deps.items():
            self.seen[eng][k] = v
        return list(deps.items())

    def op(self, eng, fn, reads=(), writes=()):
        waits = self._deps(eng, reads, writes)
        self.cnt[eng] += 1
        c = self.cnt[eng]
        self.ops[eng].append((waits, fn, (eng, 1)))
        for t in reads:
            t.r[eng] = c
        for t in writes:
            t.w = (eng, c)
            t.r = {}
        return c

    def dma(self, q, fn, reads=(), writes=()):
        s = self.dma_rr[q]
        self.dma_rr[q] = (s + 1) % self.n_dma
        key = "d%s%d" % (q, s)
        s = key
        waits = self._deps(q, reads, writes, is_dma=True)
        prev = self.dma_val.get(s, 0)
        if prev > 0 and self.seen[q].get(key, 0) < prev:
            waits.append((key, prev))
            self.seen[q][key] = prev
        self.dma_val[s] = prev + 16
        v = self.dma_val[s]
        self.ops[q].append((waits, fn, (key, 16)))
        for t in reads:
            t.r[key] = v
        for t in writes:
            t.w = (key, v)
            t.r = {}

    def finish(self, eng, tiles):
        waits = [t.w for t in tiles if t.w is not None]
        self.final_waits.append((eng, waits))

    def emit(self, stack):
        nc = self.nc
        keys = list(self.ENGS) + sorted(self.dma_val.keys())
        for k in keys:
            self.sems[k] = stack.enter_context(nc.semaphore("s_" + k))
        block = stack.enter_context(nc.Block())
        sems = self.sems

        def run(eng_name):
            def body(e):
                for waits, fn, inc in self.ops[eng_name]:
                    for k, v in waits:
                        e.wait_ge(sems[k], v)
                    ins = fn(e)
                    ins.then_inc(sems[inc[0]], inc[1])
                for en, waits in self.final_waits:
                    if en == eng_name:
                        for k, v in waits:
                            e.wait_ge(sems[k], v)
            return body

        block.tensor(run("pe"))
        block.scalar(run("act"))
        block.vector(run("dve"))
        block.gpsimd(run("pool"))
        block.sync(run("sp"))


def build_program(stage=99):
    nc = bass.Bass("TRN2", target_bir_lowering=False, dynamic_dma_scratch_size=2048)
    dt = nc.dram_tensor
    x_d = dt("x", [SEQ, D], F32, kind="ExternalInput").ap()
    meta_d = dt("meta", [16, D], F32, kind="ExternalInput").ap()
    ws_d = dt("ws", [NSLOT, 128, 4096], F32, kind="ExternalInput").ap()
    wd_d = dt("wd", [2, 128, NJ * 1024], F32, kind="ExternalInput").ap()
    gcol_d = dt("gcol", [128, 24], F32, kind="ExternalInput").ap()
    gfin_d = dt("gfin", [D], F32, kind="ExternalInput").ap()
    sink_d = dt("sink", [8], F32, kind="ExternalInput").ap()
    cs_d = dt("cs", [NBLK * 128, 64], F32, kind="ExternalInput").ap()
    cst_d = dt("cst", [128, 1152], F32, kind="ExternalInput").ap()
    p2_d = dt("p2", [128, 32], F32, kind="ExternalInput").ap()
    wsb_d = dt("wsb", [NSLOT, 128, 4096], BF16, kind="Internal").ap()
    wdb_d = dt("wdb", [2, 128, NJ * 1024], BF16, kind="Internal").ap()
    out_d = dt("out", [SEQ, D], F32, kind="ExternalOutput").ap()

    with ExitStack() as st:
        def sb(n, s, d):
            return st.enter_context(nc.sbuf_tensor(n, s, d))

        def ps(n, s, d):
            return st.enter_context(nc.psum_tensor(n, s, d))

        PA = ps("PA", [128, 4, 512], F32)
        PB = ps("PB", [128, 2, 512], F32)
        PTR = ps("PTR", [128, 2, 1024], BF16)

        X = sb("X", [128, 4, 1024], F32)
        XT = sb("XT", [128, 8, 512], BF16)
        BIG = sb("BIG", [128, 7168], F32)
        AT = BIG[:].bitcast(BF16)
        SG = sb("SG", [128, 2, 512], BF16)
        WR = sb("WR", [128, 3, 4096], BF16)
        WD = sb("WD", [128, NJ * 1024], BF16)
        SGA = sb("SGA", [128, 4, 1024], BF16)
        SGB = sb("SGB", [128, 4, 1024], BF16)
        ROT = sb("ROT", [128, 28, 64], BF16)
        QT = sb("QT", [128, 14, 128], BF16)
        KST = sb("KST", [128, 2, 2, 128], BF16)
        KAT = sb("KAT", [128, NBLK * 128], BF16)
        KIT = sb("KIT", [128, NBLK * 128], BF16)
        VA = sb("VA", [128, NBLK, 66], BF16)
        VS = sb("VS", [128, 5, 2, 66], BF16)
        SC = sb("SC", [128, NBLK * 128], F32)
        NEG = sb("NEG", [128, NBLK * 128], BF16)
        RL = sb("RL", [128, 4, 512], F32)
        PT = sb("PT", [128, 2, 1024], BF16)
        OSP = sb("OSP", [128, 512], BF16)
        OWP = sb("OWP", [128, 512], BF16)
        OT = sb("OT", [128, 8, 128], BF16)
        MG = sb("MG", [128, 1024], BF16)
        CAUSF = sb("CAUSF", [128, 128], F32)
        CB = sb("CB", [128, 1152], BF16)
        GFIN = sb("GFIN", [128, 1024], F32)
        CS = sb("CS", [128, 4, 64], F32)
        G = sb("G", [128, 24], F32)
        P2 = sb("P2", [128, 32], F32)
        SM = sb("SM", [128, 64], F32)
        WT = sb("WT", [128, 32], F32)
        WI = sb("WI", [128, 4, 4], F32)
        REC = sb("REC", [128, 8], F32)
        ESINK = sb("ESINK", [128, 8], F32)

        SS = SM[:, 0:4]
        MS = SM[:, 4:8]
        RSTD = SM[:, 8:12]
        MHALF = SM[:, 12:16]
        AMAX = SM[:, 16:17]
        LO = SM[:, 17:18]
        RNG = SM[:, 18:19]
        MID = SM[:, 19:20]
        CNT = SM[:, 20:21]
        TV = SM[:, 21:22]
        THR = SM[:, 22:23]
        DEN = SM[:, 24:32]
        SA = SM[:, 32:33]

        NEG1 = WD[:, 0:4224]
        QT1 = WD[:, 4224:6016].rearrange("p (g t) -> p g t", g=14)
        KSTW = WD[:, 6016:7040].rearrange("p (a k t) -> p a k t", a=4, k=2)
        QT2 = WD[:, 7040:8832].rearrange("p (g t) -> p g t", g=14)
        QTs = [QT, QT1, QT2]
        OSPW = WD[:, 8832:10880].rearrange("p (i c) -> p i c", i=4)
        OWPW = WD[:, 10880:12928].rearrange("p (i c) -> p i c", i=4)
        OT2 = WD[:, 12928:13952].rearrange("p (k t) -> p k t", k=8)
        MG2 = WD[:, 13952:14976]
        OTs = [OT[:], OT2]
        MGs = [MG[:], MG2]
        NEGs = [NEG, NEG1]
        S = Sched(nc)
        T = TT
        tX = [T("X%d" % i) for i in range(4)]
        tXT = T("XT")
        tATc = [T("AT%d" % j) for j in range(NJ)]
        tR = [T("R%d" % i) for i in range(4)]
        tSG = [T("SG0"), T("SG1")]
        tWR = [T("WR%d" % i) for i in range(3)]
        tWD = T("WD")
        tSGA = [T("SGA%d" % i) for i in range(4)]
        tSGB = [T("SGB%d" % i) for i in range(4)]
        tROT = T("ROT")
        tQTs = [T("QT0"), T("QT1"), T("QT2")]
        tNEGs = [T("NEG0"), T("NEG1")]
        tNEGAs = [T("NEGA0"), T("NEGA1")]
        tKSTW = [T("KSTW%d" % i) for i in range(4)]
        tOSPs = [T("OSP%d" % i) for i in range(4)]
        tOWPs = [T("OWP%d" % i) for i in range(4)]
        tOTs = [T("OT0"), T("OT1")]
        tMGs = [T("MG0"), T("MG1")]
        tKST0 = T("KST0")
        tKAT, tKIT, tVA = T("KAT"), T("KIT"), T("VA")
        tVS = [T("VS%d" % i) for i in range(5)]
        tSC = T("SC")
        tSA, tMID = T("SA"), T("MID")
        tRL = [T("RL%d" % i) for i in range(4)]
        tPT = [T("PT0"), T("PT1")]
        tOSP, tOWP, tOT, tMG = T("OSP"), T("OWP"), T("OT"), T("MG")
        tC = T("CONST")
        tCS = T("CS")
        tSM = T("SM")
        tBI = T("BI")
        tWI = T("WI")
        tREC = T("REC")
        tPA = [T("PA%d" % i) for i in range(4)]
        tPB = [T("PB0"), T("PB1")]
        tPTR = [T("PTR0"), T("PTR1")]
        tWSB = [T("wsb%d" % i) for i in range(NSLOT)]
        tWDB = [T("wdb0"), T("wdb1")]
        tOUT = [T("out%d" % i) for i in range(40)]
        tF = [T("F%d" % i) for i in range(4)]

        CF = RL[:].rearrange("p a b -> p (a b)")[:, 0:1152]
        S.dma("sp", lambda e: e.dma_start(out=CF, in_=cst_d), writes=[tC] + tRL)
        S.dma("sp", lambda e: e.dma_start(out=CAUSF[:], in_=cst_d[:, 512:640]), writes=[tC])
        S.dma("sp", lambda e: e.dma_start(out=G[:], in_=gcol_d), writes=[tC])
        S.dma("sp", lambda e: e.dma_start(out=P2[:], in_=p2_d), writes=[tC])
        S.dma("sp", lambda e: e.dma_start(out=GFIN[:], in_=gfin_d.partition_broadcast(128)), writes=[tC])
        S.dma("sp", lambda e: e.dma_start(out=ESINK[:], in_=sink_d.partition_broadcast(128)), writes=[tC])
        S.op("dve", lambda e: e.tensor_copy(out=CB[:], in_=CF), reads=[tC] + tRL, writes=[tC])
        S.op("act", lambda e: e.activation(out=ESINK[:], in_=ESINK[:], func=AF.Exp), reads=[tC], writes=[tC])
        S.op("pool", lambda e: e.memset(SM[:, 12:16], -0.5), writes=[tSM])
        S.op("pool", lambda e: e.memset(VA[:, :, 64:66], 1.0), writes=[tVA])
        S.op("pool", lambda e: e.memset(VS[:, :, :, 64:66], 1.0), writes=tVS)
        ID4 = CB[:, 0:512]
        CAUS = CAUSF[:]
        SWCUR = CB[:, 640:768]
        SWPREV = CB[:, 768:896]
        SWPREV1 = CB[:, 896:1024]

        def convert_group(conv_order, first_reads=()):
          fr = list(first_reads)
          for it in conv_order:
            if isinstance(it, str):
                f = int(it[2])
                for hlf in range(2):
                    sl = slice(hlf * 11 * 1024, (hlf + 1) * 11 * 1024)
                    S.dma("pool", lambda e, f=f, sl=sl: e.dma_start(out=wdb_d[f, :, sl], in_=wd_d[f, :, sl], max_dma_last_dim=8192), writes=[tWDB[f]])
            else:
                S.dma("pool", lambda e, it=it: e.dma_start(out=wsb_d[it], in_=ws_d[it], max_dma_last_dim=8192), reads=fr, writes=[tWSB[it]])
                fr = []

        tiles = [[0]] + [list(range(1 + 4 * t, 5 + 4 * t)) for t in range(8)]
        seq = []
        for ti, blks in enumerate(tiles):
            if ti == 0:
                seq += list(range(0, 15))
            else:
                seq += list(range(0, NSLOT))
        ring = {"issued": 0, "used": 0, "holds": set()}

        def issue_next():
            k = ring["issued"]
            if k >= len(seq):
                return
            pos = k % 3
            slot = seq[k]
            S.dma("sp", lambda e, pos=pos, slot=slot: e.dma_start(out=WR[:, pos, :], in_=wsb_d[slot]),
                  reads=[tWSB[slot]], writes=[tWR[pos]])
            ring["issued"] += 1

        def pump():
            k = ring["used"] - 1
            released = min([k] + list(ring["holds"]))
            while ring["issued"] < min(k + 3, released + 3, len(seq)):
                issue_next()

        def use_slot(expect, hold=False):
            k = ring["used"]
            assert seq[k] == expect, (k, seq[k], expect)
            ring["used"] += 1
            if hold:
                ring["holds"].add(k)
            pump()
            assert ring["issued"] > k
            return k % 3

        def unhold_all():
            ring["holds"].clear()
            pump()

        def load_wd(f, alias=False):
            wr = [tWD] + ((tQTs + tNEGs + tNEGAs + tKSTW + tOSPs + tOWPs + [tOTs[1], tMGs[1]]) if alias else [])
            S.dma("pool", lambda e, f=f: e.dma_start(out=WD[:], in_=wdb_d[f]), reads=[tWDB[f]], writes=wr)

        SGf = SG[:].rearrange("p a b -> p (a b)")
        XSs = [PT[:, 0, :], PT[:, 1, :]]

        def norm_to_xt(NB, gi, ptr_par):
            for i in range(NB):
                S.op("act", lambda e, i=i: e.activation(out=SGf, in_=X[:, i, :], func=AF.Square, accum_out=SS[:, i:i + 1]),
                     reads=[tX[i]], writes=[tSG[0], tSG[1], tSM])
            S.op("dve", lambda e: e.tensor_scalar(out=MS[:, 0:NB], in0=SS[:, 0:NB], scalar1=1.0 / D, scalar2=EPS, op0=ALU.mult, op1=ALU.add),
                 reads=[tSM], writes=[tSM])
            S.op("pool", lambda e: e.tensor_tensor(out=RSTD[:, 0:NB], in0=MS[:, 0:NB], in1=MHALF[:, 0:NB], op=ALU.pow),
                 reads=[tSM], writes=[tSM])
            for i in range(NB):
                xs, txs = XSs[i % 2], tPT[i % 2]
                S.op("act", lambda e, i=i, xs=xs: e.mul(out=xs, in_=X[:, i, :], mul=RSTD[:, i:i + 1]), reads=[tX[i], tSM], writes=[txs])
                par = (ptr_par + i) % 2

                def tr(e, par=par, xs=xs):
                    ins = None
                    for kc in range(8):
                        ins = e.transpose(out=PTR[:, par, kc * 128:(kc + 1) * 128], in_=xs[:, kc * 128:(kc + 1) * 128], identity=CB[:, 0:128])
                    return ins
                S.op("pe", tr, reads=[txs, tC], writes=[tPTR[par]])
                S.op("dve", lambda e, i=i, par=par: e.tensor_tensor(
                    out=XT[:, :, i * 128:(i + 1) * 128],
                    in0=PTR[:, par, :].rearrange("p (k t) -> p k t", k=8),
                    in1=G[:, gi * 8:(gi + 1) * 8].unsqueeze(2).to_broadcast([128, 8, 128]), op=ALU.mult),
                    reads=[tPTR[par], tC], writes=[tXT])

        def ffn(f, NB, alias_tiles):
            TK = NB * 128
            base = 0 if f == 0 else 23
            first = True
            for j in range(NJ):
                if j % 2 == 0:
                    pos = use_slot(base + j // 2)
                cj = j % 2
                gp = j % 2
                woff = cj * 2048

                def gu(e, pos=pos, woff=woff, gp=gp):
                    ins = None
                    for g in range(2):
                        for kc in range(8):
                            o = woff + g * 1024 + kc * 128
                            ins = e.matmul(PA[:, g * 2 + gp, 0:TK], lhsT=WR[:, pos, o:o + 128], rhs=XT[:, kc, 0:TK], start=(kc == 0), stop=(kc == 7))
                    return ins
                S.op("pe", gu, reads=[tWR[pos], tXT], writes=[tPA[gp], tPA[2 + gp]])
                S.op("act", lambda e, gp=gp: e.activation(out=SG[:, gp, 0:TK], in_=PA[:, gp, 0:TK], func=AF.Silu), reads=[tPA[gp]], writes=[tSG[gp]])
                wr = [tATc[j]] + (alias_tiles if first else [])
                first = False
                S.op("dve", lambda e, j=j, gp=gp: e.tensor_tensor(out=AT[:, j * 512:j * 512 + TK], in0=PA[:, 2 + gp, 0:TK], in1=SG[:, gp, 0:TK], op=ALU.mult),
                     reads=[tPA[2 + gp], tSG[gp]], writes=wr)
            n = 0
            for i in range(NB):
                for half in range(2):
                    par = n % 2
                    n += 1

                    def dn(e, i=i, half=half, par=par):
                        ins = None
                        for j in range(NJ):
                            ins = e.matmul(PB[:, par, :], lhsT=AT[:, j * 512 + i * 128:j * 512 + (i + 1) * 128],
                                           rhs=WD[:, j * 1024 + half * 512:j * 1024 + (half + 1) * 512], start=(j == 0), stop=(j == NJ - 1))
                        return ins
                    S.op("pe", dn, reads=tATc + [tWD], writes=[tPB[par]])
                    S.op("dve", lambda e, i=i, half=half, par=par: e.scalar_tensor_tensor(
                        out=X[:, i, half * 512:(half + 1) * 512], in0=PB[:, par, :], scalar=0.5, in1=X[:, i, half * 512:(half + 1) * 512],
                        op0=ALU.mult, op1=ALU.add), reads=[tPB[par], tX[i]], writes=[tX[i]])

        Rv = BIG[:].rearrange("p (i c) -> p i c", i=4)

        def project(NB, blks, chunks):
            n = 0
            for cc in chunks:
                pos = use_slot(11 + cc)
                for i in range(NB):
                    par = n % 2
                    n += 1
                    c = blks[i]

                    def pj(e, pos=pos, i=i, par=par):
                        ins = None
                        for kc in range(8):
                            ins = e.matmul(PB[:, par, :], lhsT=XT[:, kc, i * 128:(i + 1) * 128], rhs=WR[:, pos, kc * 512:(kc + 1) * 512], start=(kc == 0), stop=(kc == 7))
                        return ins
                    S.op("pe", pj, reads=[tWR[pos], tXT], writes=[tPB[par]])
                    if cc < 3:
                        wr = [tR[i]] + (tATc if cc == 0 and i == 0 else [])
                        eng = "act" if (n % 2 == 0) else "dve"
                        if eng == "act":
                            S.op("act", lambda e, i=i, cc=cc, par=par: e.copy(out=Rv[:, i, cc * 512:(cc + 1) * 512], in_=PB[:, par, :]), reads=[tPB[par]], writes=wr)
                        else:
                            S.op("dve", lambda e, i=i, cc=cc, par=par: e.tensor_copy(out=Rv[:, i, cc * 512:(cc + 1) * 512], in_=PB[:, par, :]), reads=[tPB[par]], writes=wr)
                    elif cc == 3:
                        S.op("act", lambda e, i=i, par=par: e.copy(out=Rv[:, i, 1536:1792], in_=PB[:, par, 0:256]), reads=[tPB[par]], writes=[tR[i]])
                        S.op("act", lambda e, c=c, par=par: e.copy(out=VA[:, c, 0:64], in_=PB[:, par, 256:320]), reads=[tPB[par]], writes=[tVA])
                        S.op("act", lambda e, i=i, par=par: e.copy(out=VS[:, i + 1, :, 0:64], in_=PB[:, par, 320:448].rearrange("p (k d) -> p k d", k=2)),
                             reads=[tPB[par]], writes=[tVS[i + 1]])
                        S.op("act", lambda e, i=i, par=par: e.mul(out=WI[:, i, :], in_=PB[:, par, 448:452], mul=IDX_SCALE),
                             reads=[tPB[par]], writes=[tWI])
                    else:
                        gsel = (cc - 4) // 2
                        hh = (cc - 4) % 2
                        dst = SGA if gsel == 0 else SGB
                        tdst = tSGA if gsel == 0 else tSGB
                        S.op("act", lambda e, dst=dst, i=i, hh=hh, par=par: e.activation(out=dst[:, i, hh * 512:(hh + 1) * 512], in_=PB[:, par, :], func=AF.Sigmoid),
                             reads=[tPB[par]], writes=[tdst[i]])
                    yield

        T1 = RL[:, 0:2, :].rearrange("p a b -> p (a b)")[:, 0:896].rearrange("p (h d) -> p h d", h=28)
        T2 = RL[:, 2:4, :].rearrange("p a b -> p (a b)")[:, 0:896].rearrange("p (h d) -> p h d", h=28)

        def rope_block(i, c, tile0=False):
            QTx, tQ = QTs[i % 3], tQTs[i % 3]
            Rb = Rv[:, i, :].rearrange("p (h t d) -> p h t d", h=28, t=2)
            x1 = Rb[:, :, 0, :]
            x2 = Rb[:, :, 1, :]
            cos = CS[:, i, 0:32].unsqueeze(1).to_broadcast([128, 28, 32])
            sin = CS[:, i, 32:64].unsqueeze(1).to_broadcast([128, 28, 32])
            rl01, rl23 = [tRL[0], tRL[1]], [tRL[2], tRL[3]]
            P = "pool"
            S.op(P, lambda e: e.tensor_tensor(out=T1, in0=x1, in1=cos, op=ALU.mult), reads=[tR[i], tCS], writes=rl01)
            S.op(P, lambda e: e.tensor_tensor(out=T2, in0=x2, in1=sin, op=ALU.mult), reads=[tR[i], tCS], writes=rl23)
            S.op(P, lambda e: e.tensor_tensor(out=ROT[:, :, 0:32], in0=T1, in1=T2, op=ALU.subtract), reads=rl01 + rl23, writes=[tROT])
            S.op(P, lambda e: e.tensor_tensor(out=T1, in0=x2, in1=cos, op=ALU.mult), reads=[tR[i], tCS], writes=rl01)
            S.op(P, lambda e: e.tensor_tensor(out=T2, in0=x1, in1=sin, op=ALU.mult), reads=[tR[i], tCS], writes=rl23)
            S.op(P, lambda e: e.tensor_tensor(out=ROT[:, :, 32:64], in0=T1, in1=T2, op=ALU.add), reads=rl01 + rl23, writes=[tROT])
            ROTf = ROT[:].rearrange("p h d -> p (h d)")

            def tr(e):
                ins = None
                for g in range(14):
                    ins = e.transpose(out=PTR[:, g // 8, (g % 8) * 128:(g % 8 + 1) * 128], in_=ROTf[:, g * 128:(g + 1) * 128], identity=CB[:, 0:128])
                return ins
            S.op("pe", tr, reads=[tROT, tC], writes=[tPTR[0], tPTR[1]])
            S.op("act", lambda e: e.copy(out=QTx[:, 0:8, :], in_=PTR[:, 0, :].rearrange("p (g t) -> p g t", g=8)), reads=[tPTR[0]], writes=[tQ])
            S.op("dve", lambda e: e.tensor_copy(out=QTx[:, 8:14, :], in_=PTR[:, 1, 0:768].rearrange("p (g t) -> p g t", g=6)), reads=[tPTR[1]], writes=[tQ])
            S.op("pool", lambda e: e.tensor_copy(out=KAT[:, c * 128:(c + 1) * 128], in_=QTx[:, 6, :]), reads=[tQ], writes=[tKAT])
            S.op("pool", lambda e: e.tensor_copy(out=KIT[:, c * 128:(c + 1) * 128], in_=QTx[:, 7, :]), reads=[tQ], writes=[tKIT])
            if tile0 or i == 3:
                S.op("pool", lambda e: e.tensor_copy(out=KST[:, 0, :, :], in_=QTx[:, 12:14, :]), reads=[tQ], writes=[tKST0])
            if not tile0:
                S.op("pool", lambda e: e.tensor_copy(out=KSTW[:, i, :, :], in_=QTx[:, 12:14, :]), reads=[tQ], writes=[tKSTW[i]])

        def attn_core(keyblocks, out_sb, t_out, sink, QTx, tQ):
            nk = len(keyblocks)

            def emit_sc(jj):
                s = jj % 2
                kfn, negap, vfn, rds, swa = keyblocks[jj]

                def sc(e):
                    ins = None
                    for hf in range(2):
                        if not swa:
                            e.matmul(PA[:, 2 * s + hf, :], lhsT=kfn(hf, 0), rhs=QTx[hf * 64:(hf + 1) * 64, 0:4, :], start=True, stop=False)
                        else:
                            for kap in range(2):
                                e.matmul(PA[:, 2 * s + hf, kap * 256:(kap + 1) * 256], lhsT=kfn(hf, kap),
                                         rhs=QTx[hf * 64:(hf + 1) * 64, 8 + 2 * kap:10 + 2 * kap, :], start=(kap == 0), stop=False, skip_group_check=True)
                    for hf in range(2):
                        ins = e.matmul(PA[:, 2 * s + hf, :], lhsT=negap, rhs=ID4, start=False, stop=True, skip_group_check=True)
                    return ins
                S.op("pe", sc, reads=rds + [tQ, tC], writes=[tPA[2 * s], tPA[2 * s + 1]])

            def emit_exp_pv(jj):
                s = jj % 2
                kfn, negap, vfn, rds, swa = keyblocks[jj]
                S.op("act", lambda e: e.activation(out=PT[:, s, :], in_=PA[:, 2 * s:2 * s + 2, :].rearrange("p a b -> p (a b)"), func=AF.Exp, scale=0.125),
                     reads=[tPA[2 * s], tPA[2 * s + 1]], writes=[tPT[s]])

                def pv(e):
                    ins = None
                    for hf in range(2):
                        for pr in range(4):
                            ins = e.matmul(PB[:, hf, pr * 65:(pr + 1) * 65], lhsT=PT[:, s, (hf * 4 + pr) * 128:(hf * 4 + pr + 1) * 128], rhs=vfn(pr),
                                           start=(jj == 0 and pr == 0), stop=(jj == nk - 1 and pr == 3), skip_group_check=True)
                    return ins
                S.op("pe", pv, reads=rds + [tPT[s]], writes=[tPB[0], tPB[1]])

            emit_sc(0)
            for jj in range(nk):
                if jj + 1 < nk:
                    emit_sc(jj + 1)
                emit_exp_pv(jj)
                yield
            PBv = PB[:, :, 0:260].rearrange("p b (r e) -> p b r e", e=65)
            RECv = REC[:].rearrange("p (b r o) -> p b r o", b=2, o=1)
            if sink:
                S.op("dve", lambda e: e.tensor_tensor(out=RECv, in0=PBv[:, :, :, 64:65], in1=ESINK[:].rearrange("p (b r o) -> p b r o", b=2, o=1), op=ALU.add),
                     reads=[tPB[0], tPB[1], tC], writes=[tREC])
                S.op("dve", lambda e: e.reciprocal(out=REC[:], in_=REC[:]), reads=[tREC], writes=[tREC])
            else:
                S.op("dve", lambda e: e.reciprocal(out=RECv, in_=PBv[:, :, :, 64:65]), reads=[tPB[0], tPB[1]], writes=[tREC])
            S.op("dve", lambda e: e.tensor_tensor(out=out_sb[:].rearrange("p (b r d) -> p b r d", b=2, r=4), in0=PBv[:, :, :, 0:64],
                                                  in1=RECv.to_broadcast([128, 2, 4, 64]), op=ALU.mult),
                 reads=[tPB[0], tPB[1], tREC], writes=[t_out])

        def index_bisect(i, c, frac=0.42):
            split = frac < 1.0
            QTx, tQ = QTs[i % 3], tQTs[i % 3]
            NEGx, tNEG, tNEGA = NEGs[i % 2], tNEGs[i % 2], tNEGAs[i % 2]
            n = (c + 1) * 128
            nch = (n + 511) // 512
            for ch in range(nch):
                w = min(512, n - ch * 512)

                for pr in range(2):
                    def ix(e, ch=ch, w=w, pr=pr):
                        ins = None
                        for hf in range(2):
                            ins = e.matmul(PA[:, 2 * pr + hf, 0:w], lhsT=QTx[hf * 64:(hf + 1) * 64, 4 + pr, :],
                                           rhs=KIT[hf * 64:(hf + 1) * 64, ch * 512:ch * 512 + w], start=True, stop=True)
                        return ins
                    S.op("pe", ix, reads=[tQ, tKIT], writes=[tPA[2 * pr], tPA[2 * pr + 1]])
                for h in range(4):
                    S.op("act", lambda e, h=h, w=w: e.activation(out=RL[:, h, 0:w], in_=PA[:, h, 0:w], func=AF.Relu), reads=[tPA[h]], writes=[tRL[h]])
                S.op("dve", lambda e, ch=ch, w=w: e.tensor_scalar(out=SC[:, ch * 512:ch * 512 + w], in0=RL[:, 0, 0:w], scalar1=WI[:, i, 0:1], scalar2=None, op0=ALU.mult),
                     reads=[tRL[0], tWI], writes=[tSC])
                for h in range(1, 4):
                    S.op("dve", lambda e, ch=ch, w=w, h=h: e.scalar_tensor_tensor(out=SC[:, ch * 512:ch * 512 + w], in0=RL[:, h, 0:w], scalar=WI[:, i, h:h + 1],
                                                                                 in1=SC[:, ch * 512:ch * 512 + w], op0=ALU.mult, op1=ALU.add),
                         reads=[tRL[h], tWI, tSC], writes=[tSC])
            V = "dve"
            S.op(V, lambda e: e.tensor_reduce(out=AMAX, in_=SC[:, 0:n], axis=AX.X, op=ALU.max, apply_absolute_value=True), reads=[tSC], writes=[tBI])
            S.op(V, lambda e: e.memset(SC[:, 0:112], -1e30), reads=[tSC], writes=[tSC])
            S.op(V, lambda e: e.tensor_tensor(out=SC[:, c * 128:(c + 1) * 128], in0=SC[:, c * 128:(c + 1) * 128], in1=CAUS, op=ALU.add), reads=[tSC, tC], writes=[tSC])
            S.op(V, lambda e: e.tensor_scalar(out=LO, in0=AMAX, scalar1=-1.001, scalar2=-1e-20, op0=ALU.mult, op1=ALU.add), reads=[tBI], writes=[tBI])
            S.op(V, lambda e: e.tensor_tensor(out=RNG, in0=AMAX, in1=LO, op=ALU.subtract), reads=[tBI], writes=[tBI])
            S.op(V, lambda e: e.tensor_scalar(out=WT[:, 0:NIT + 1], in0=P2[:, 0:NIT + 1], scalar1=RNG, scalar2=None, op0=ALU.mult), reads=[tBI, tC], writes=[tBI])
            S.op(V, lambda e: e.tensor_tensor(out=MID, in0=LO, in1=WT[:, 0:1], op=ALU.add), reads=[tBI], writes=[tMID])
            nd = (min(c, max(1, int(frac * (c + 1) + 0.5))) * 128) if split else n
            na = n - nd
            for it in range(NIT):
                if split:
                    S.op("act", lambda e: e.activation(out=NEGx[:, nd:n], in_=SC[:, nd:n], func=AF.Sign, bias=MID, scale=-1.0, accum_out=SA),
                         reads=[tSC, tMID], writes=[tNEGA, tSA])
                S.op(V, lambda e: e.tensor_scalar(out=NEGx[:, 0:nd], in0=SC[:, 0:nd], scalar1=MID, scalar2=None, op0=ALU.is_gt, op1=ALU.add, accum_out=CNT),
                     reads=[tSC, tMID], writes=[tNEG, tBI])
                if split:
                    S.op(V, lambda e: e.scalar_tensor_tensor(out=CNT, in0=SA, scalar=-0.5, in1=CNT, op0=ALU.mult, op1=ALU.add), reads=[tSA, tBI], writes=[tBI])
                S.op(V, lambda e: e.tensor_scalar(out=TV, in0=CNT, scalar1=255.5 - na / 2.0, scalar2=0.5, op0=ALU.is_gt, op1=ALU.subtract), reads=[tBI], writes=[tBI])
                S.op(V, lambda e, it=it: e.scalar_tensor_tensor(out=MID, in0=TV, scalar=WT[:, it:it + 1], in1=MID, op0=ALU.mult, op1=ALU.add), reads=[tBI, tMID], writes=[tMID])
                yield
            S.op(V, lambda e: e.tensor_tensor(out=THR, in0=MID, in1=WT[:, NIT:NIT + 1], op=ALU.subtract), reads=[tBI, tMID], writes=[tBI])
            S.op(V, lambda e: e.tensor_scalar(out=NEGx[:, 0:n], in0=SC[:, 0:n], scalar1=THR, scalar2=NEGM, op0=ALU.is_le, op1=ALU.mult), reads=[tSC, tBI], writes=[tNEG, tNEGA])

        def attn_main(i, c):
            QTx, tQ = QTs[i % 3], tQTs[i % 3]
            NEGx, tNEG, tNEGA = NEGs[i % 2], tNEGs[i % 2], tNEGAs[i % 2]
            kbs = []
            for j in range(c + 1):
                kbs.append((lambda hf, kap, j=j: KAT[hf * 64:(hf + 1) * 64, j * 128:(j + 1) * 128], NEGx[:, j * 128:(j + 1) * 128],
                            lambda pr, j=j: VA[:, j, 0:65], [tKAT, tNEG, tNEGA, tVA], False))
            yield from attn_core(kbs, OSPW[:, i, :], tOSPs[i], False, QTx, tQ)
            if i == 0:
                kprev = lambda hf, kap: KST[hf * 64:(hf + 1) * 64, 0, kap, :]
                tkp = tKST0
            else:
                kprev = lambda hf, kap: KSTW[hf * 64:(hf + 1) * 64, i - 1, kap, :]
                tkp = tKSTW[i - 1]
            kbs = [
                (kprev, SWPREV1 if c == 1 else SWPREV, lambda pr: VS[:, i, pr // 2, 0:65], [tkp, tVS[i], tC], True),
                (lambda hf, kap: KSTW[hf * 64:(hf + 1) * 64, i, kap, :], SWCUR, lambda pr: VS[:, i + 1, pr // 2, 0:65], [tKSTW[i], tVS[i + 1], tC], True),
            ]
            yield from attn_core(kbs, OWPW[:, i, :], tOWPs[i], True, QTx, tQ)

        def tail_a(i):
            par = i % 2
            OTx, tOTx = OTs[i % 2], tOTs[i % 2]

            def tro(e):
                ins = None
                for k in range(4):
                    e.transpose(out=PTR[:, par, k * 128:(k + 1) * 128], in_=OSPW[:, i, k * 128:(k + 1) * 128], identity=CB[:, 0:128])
                for k in range(4):
                    ins = e.transpose(out=PTR[:, par, (4 + k) * 128:(5 + k) * 128], in_=OWPW[:, i, k * 128:(k + 1) * 128], identity=CB[:, 0:128])
                return ins
            S.op("pe", tro, reads=[tOSPs[i], tOWPs[i], tC], writes=[tPTR[par]])
            S.op("act", lambda e: e.copy(out=OTx, in_=PTR[:, par, :].rearrange("p (k t) -> p k t", k=8)), reads=[tPTR[par]], writes=[tOTx])

        def tail_b(i, pos_bs, pos_bw):
            OTx, tOTx = OTs[i % 2], tOTs[i % 2]
            MGx, tMGx = MGs[i % 2], tMGs[i % 2]

            def br(e):
                ins = None
                for b, pos in enumerate((pos_bs, pos_bw)):
                    for half in range(2):
                        for kc in range(4):
                            ins = e.matmul(PA[:, 2 * b + half, :], lhsT=OTx[:, 4 * b + kc, :], rhs=WR[:, pos, kc * 1024 + half * 512:kc * 1024 + (half + 1) * 512],
                                           start=(kc == 0), stop=(kc == 3))
                return ins
            S.op("pe", br, reads=[tOTx, tWR[pos_bs], tWR[pos_bw]], writes=tPA)
            M1 = RL[:, 0:2, :]
            M2 = RL[:, 2:4, :]
            S.op("dve", lambda e: e.tensor_tensor(out=M1, in0=PA[:, 0:2, :], in1=SGA[:, i, :].rearrange("p (a b) -> p a b", a=2), op=ALU.mult),
                 reads=[tPA[0], tPA[1], tSGA[i]], writes=[tRL[0], tRL[1]])
            S.op("dve", lambda e: e.tensor_tensor(out=M2, in0=PA[:, 2:4, :], in1=SGB[:, i, :].rearrange("p (a b) -> p a b", a=2), op=ALU.mult),
                 reads=[tPA[2], tPA[3], tSGB[i]], writes=[tRL[2], tRL[3]])
            S.op("pool", lambda e: e.tensor_tensor(out=MGx.rearrange("p (a b) -> p a b", a=2), in0=M1, in1=M2, op=ALU.add), reads=tRL, writes=[tMGx])

        def tail_c(i):
            MGx, tMGx = MGs[i % 2], tMGs[i % 2]
            par2 = (i + 1) % 2

            def trm(e):
                ins = None
                for k in range(8):
                    ins = e.transpose(out=PTR[:, par2, k * 128:(k + 1) * 128], in_=MGx[:, k * 128:(k + 1) * 128], identity=CB[:, 0:128])
                return ins
            S.op("pe", trm, reads=[tMGx, tC], writes=[tPTR[par2]])
            S.op("act", lambda e: e.copy(out=XT[:, :, i * 128:(i + 1) * 128], in_=PTR[:, par2, :].rearrange("p (k t) -> p k t", k=8)), reads=[tPTR[par2]], writes=[tXT])

        def out_proj(NB, pos_a, pos_b):
            n = 0
            for i in range(NB):
                for half in range(2):
                    par = n % 2
                    n += 1

                    def wo(e, i=i, half=half, par=par):
                        ins = None
                        for kc in range(8):
                            pos = pos_a if kc < 4 else pos_b
                            o = (kc % 4) * 1024 + half * 512
                            ins = e.matmul(PB[:, par, :], lhsT=XT[:, kc, i * 128:(i + 1) * 128], rhs=WR[:, pos, o:o + 512], start=(kc == 0), stop=(kc == 7))
                        return ins
                    S.op("pe", wo, reads=[tXT, tWR[pos_a], tWR[pos_b]], writes=[tPB[par]])
                    S.op("dve", lambda e, i=i, half=half, par=par: e.tensor_tensor(out=X[:, i, half * 512:(half + 1) * 512], in0=PB[:, par, :],
                                                                                   in1=X[:, i, half * 512:(half + 1) * 512], op=ALU.add),
                         reads=[tPB[par], tX[i]], writes=[tX[i]])

        def final_norm_store(NB, blks, ti):
            for i in range(NB):
                tf = tF[i]
                S.op("act", lambda e, i=i: e.activation(out=SGf, in_=X[:, i, :], func=AF.Square, accum_out=SM[:, 40 + i:41 + i]), reads=[tX[i]], writes=[tSG[0], tSG[1], tf])
                S.op("dve", lambda e, i=i: e.tensor_scalar(out=SM[:, 44 + i:45 + i], in0=SM[:, 40 + i:41 + i], scalar1=1.0 / D, scalar2=EPS, op0=ALU.mult, op1=ALU.add), reads=[tf], writes=[tf])
                S.op("pool", lambda e, i=i: e.tensor_tensor(out=SM[:, 48 + i:49 + i], in0=SM[:, 44 + i:45 + i], in1=MHALF[:, 0:1], op=ALU.pow), reads=[tf, tSM], writes=[tf])
                S.op("dve", lambda e, i=i: e.scalar_tensor_tensor(out=X[:, i, :], in0=X[:, i, :], scalar=SM[:, 48 + i:49 + i], in1=GFIN[:], op0=ALU.mult, op1=ALU.mult),
                     reads=[tX[i], tf, tC], writes=[tX[i]])
                r0 = (blks[i] - 1) * 128
                S.dma("pool", lambda e, i=i, r0=r0: e.dma_start(out=out_d[r0:r0 + 128, :], in_=X[:, i, :]), reads=[tX[i]], writes=[tOUT[ti * 4 + i]])
                if ti < 8:
                    r1 = r0 + 512
                    S.dma("pool", lambda e, i=i, r1=r1: e.dma_start(out=X[:, i, :], in_=x_d[r1:r1 + 128, :]), writes=[tX[i]])

        for ti, blks in enumerate(tiles):
            NB = len(blks)
            if ti == 0:
                S.op("pool", lambda e: e.memset(X[:, 0, :], 0.0), writes=[tX[0]])
                S.dma("pool", lambda e: e.dma_start(out=X[112:128, 0, :], in_=meta_d), writes=[tX[0]])
            elif ti == 1:
                r0 = (blks[0] - 1) * 128
                S.dma("pool", lambda e, r0=r0: e.dma_start(out=X[:, 0:4, :], in_=x_d[r0:r0 + 512, :].rearrange("(i p) d -> p i d", p=128)), writes=tX)
            c0 = blks[0] * 128
            S.dma("pool", lambda e, c0=c0, NB=NB: e.dma_start(out=CS[:, 0:NB, :], in_=cs_d[c0:c0 + NB * 128, :].rearrange("(i p) d -> p i d", p=128)), writes=[tCS])
            if ti == 0:
                convert_group(list(range(0, 11)) + ["wd0"] + list(range(11, 15)))
                load_wd(0)
            norm_to_xt(NB, 0, 0)
            if ti == 1:
                convert_group(list(range(15, NSLOT)) + ["wd1"], first_reads=[tXT])
            ffn(0, NB, tR)
            norm_to_xt(NB, 1, 0)
            for _ in project(NB, blks, range(0, 4)):
                pass
            if ti == 0:
                S.op("pool", lambda e: e.tensor_copy(out=VS[:, 0, :, 0:64], in_=VS[:, 1, :, 0:64]), reads=[tVS[1]], writes=[tVS[0]])
                rope_block(0, 0, tile0=True)
                continue
            rope_block(0, blks[0])
            gb0 = index_bisect(0, blks[0], frac=0.42)
            next(gb0)
            gp = project(NB, blks, range(4, 8))
            p_done = b_done = False
            while not (p_done and b_done):
                if not p_done:
                    try:
                        next(gp)
                    except StopIteration:
                        p_done = True
                if not b_done:
                    try:
                        next(gb0)
                    except StopIteration:
                        b_done = True
            pos_bs = use_slot(19, hold=True)
            pos_bw = use_slot(20, hold=True)
            rope_block(1, blks[1])
            for i in range(NB):
                ga = attn_main(i, blks[i])
                na_steps = blks[i] + 3
                roped = (i + 2 >= NB)
                if i + 1 < NB:
                    gb = index_bisect(i + 1, blks[i + 1], frac=0.5)
                    next(gb)
                    ca, cb, a_done, b_done = 0, 1, False, False
                    while not (a_done and b_done):
                        if b_done or (not a_done and ca * NIT <= cb * na_steps):
                            try:
                                next(ga)
                                ca += 1
                            except StopIteration:
                                a_done = True
                            if not roped and ca >= na_steps // 2:
                                rope_block(i + 2, blks[i + 2])
                                roped = True
                        else:
                            try:
                                next(gb)
                                cb += 1
                            except StopIteration:
                                b_done = True
                    if not roped:
                        rope_block(i + 2, blks[i + 2])
                else:
                    for _ in ga:
                        pass
            tail_a(0)
            for i in range(NB):
                if i + 1 < NB:
                    tail_a(i + 1)
                tail_b(i, pos_bs, pos_bw)
                if i >= 1:
                    tail_c(i - 1)
            tail_c(NB - 1)
            load_wd(1, alias=True)
            S.op("pool", lambda e: e.tensor_copy(out=VS[:, 0, :, 0:64], in_=VS[:, 4, :, 0:64]), reads=[tVS[4]], writes=[tVS[0]])
            unhold_all()
            pos_a = use_slot(21, hold=True)
            pos_b = use_slot(22, hold=True)
            out_proj(NB, pos_a, pos_b)
            unhold_all()
            norm_to_xt(NB, 2, 0)
            ffn(1, NB, tR)
            if ti < 8:
                load_wd(0)
            final_norm_store(NB, blks, ti)
            if stage == 5 and ti == 1:
                break
        S.finish("pool", tOUT)
        S.emit(st)
    return nc


def _host_layout(inp):
    f32 = np.float32
    w_in = np.asarray(inp["w_in"][0], f32)
    cols = np.arange(3780)
    qa, ka, va = cols[0:512], cols[512:576], cols[576:640]
    qi, ki, wi = cols[640:896], cols[896:960], cols[960:964]
    qs, ksw, vsw = cols[964:1476], cols[1476:1604], cols[1604:1732]
    ga, gb = cols[1732:2756], cols[2756:3780]
    perm = np.concatenate([qa, qi, ka, ka, ki, ki, qs, ksw[:64], ksw[:64], ksw[64:], ksw[64:], va, vsw, wi])
    win_p = np.zeros((D, 4096), f32)
    win_p[:, :perm.size] = w_in[:, perm]
    win_p[:, 2048:3072] = w_in[:, ga]
    win_p[:, 3072:4096] = w_in[:, gb]

    ws = np.zeros((NSLOT, 128, 4096), f32)

    def gu_slots(wg, wu, base):
        wg = np.asarray(wg, f32).reshape(8, 128, NJ, 128)
        wu = np.asarray(wu, f32).reshape(8, 128, NJ, 128)
        both = np.stack([wg, wu], 0)
        both = both.reshape(2, 8, 128, 11, 2, 128)
        arr = both.transpose(3, 2, 4, 0, 1, 5)
        ws[base:base + 11] = arr.reshape(11, 128, 4096)

    gu_slots(inp["w_ffn1_gate"][0], inp["w_ffn1_up"][0], 0)
    gu_slots(inp["w_ffn2_gate"][0], inp["w_ffn2_up"][0], 23)
    wp = win_p.reshape(8, 128, 8, 512)
    ws[11:19] = wp.transpose(2, 1, 0, 3).reshape(8, 128, 4096)
    heads = [2 * (sl % 4) + sl // 4 for sl in range(8)]
    rperm = np.concatenate([np.arange(h * 64, (h + 1) * 64) for h in heads])
    for k, name in enumerate(("w_branch_sparse", "w_branch_swa")):
        wb = np.asarray(inp[name][0], f32)[rperm]
        ws[19 + k] = wb.reshape(4, 128, 1024).transpose(1, 0, 2).reshape(128, 4096)
    wo = np.asarray(inp["w_out"][0], f32).reshape(8, 128, 1024)
    ws[21] = wo[0:4].transpose(1, 0, 2).reshape(128, 4096)
    ws[22] = wo[4:8].transpose(1, 0, 2).reshape(128, 4096)

    wd = np.stack([np.asarray(inp["w_ffn1_down"][0], f32).reshape(NJ, 128, 1024).transpose(1, 0, 2).reshape(128, NJ * 1024),
                   np.asarray(inp["w_ffn2_down"][0], f32).reshape(NJ, 128, 1024).transpose(1, 0, 2).reshape(128, NJ * 1024)], 0)

    gcol = np.concatenate([np.asarray(inp[k][0], f32).reshape(8, 128).T for k in ("norm_ffn1", "norm_mix", "norm_ffn2")], 1)
    sink = np.asarray(inp["sinks"][0], f32)[heads]

    pos = np.maximum(np.arange(NBLK * 128, dtype=np.int32) - 112, 0).astype(f32)
    inv_freq = (1.0 / (np.float32(10000.0) ** (np.arange(0, 64, 2, dtype=f32) / np.float32(64)))).astype(f32)
    ang = (pos[:, None] * inv_freq[None, :]).astype(f32)
    cs = np.concatenate([np.cos(ang).astype(f32), np.sin(ang).astype(f32)], 1)

    q = np.arange(128)[:, None]
    s = np.arange(128)[None, :]
    cst = np.zeros((128, 1152), f32)
    cst[:, 0:512] = np.tile(np.eye(128, dtype=f32), (1, 4))
    cst[:, 512:640] = np.where(s <= q, 0.0, -1e30)
    cst[:, 640:768] = np.where(s <= q, 0.0, NEGM)
    cst[:, 768:896] = np.where(s > q, 0.0, NEGM)
    cst[:, 896:1024] = np.where((s > q) & (s >= 112), 0.0, NEGM)
    p2 = np.tile((2.0 ** -(np.arange(32, dtype=np.float64) + 1)).astype(f32)[None, :], (128, 1))
    shared = {"meta": np.ascontiguousarray(np.asarray(inp["meta_tokens"], f32)), "ws": ws, "wd": np.ascontiguousarray(wd),
              "gcol": np.ascontiguousarray(gcol), "gfin": np.asarray(inp["norm_final"], f32), "sink": np.ascontiguousarray(sink),
              "cs": cs, "cst": cst, "p2": p2}
    return shared


_NC_CACHE = {}


def kernel(**inputs):
    shared = _host_layout(inputs)
    x = np.asarray(inputs["x"], np.float32)
    if "nc" not in _NC_CACHE:
        _NC_CACHE["nc"] = build_program()
    nc = _NC_CACHE["nc"]
    in_maps = []
    for b in range(8):
        m = dict(shared)
        m["x"] = np.ascontiguousarray(x[b])
        in_maps.append(m)
    res = run_bass_kernel_spmd(nc, in_maps, core_ids=list(range(8)))
    return np.stack([np.asarray(r["out"], np.float32) for r in res.results], 0)
```

```python
import numpy as np
from contextlib import ExitStack
import concourse.bass as bass
import concourse.mybir as mybir
from concourse.bass_utils import run_bass_kernel_spmd

F32 = mybir.dt.float32
BF16 = mybir.dt.bfloat16
AF = mybir.ActivationFunctionType
ALU = mybir.AluOpType
AX = mybir.AxisListType

D = 1024
SEQ = 4096
NBLK = 33
FF = 2816
NJ = 22
NSLOT = 34
NIT = 16
EPS = 1e-6
IDX_SCALE = (4 ** -0.5) * (64 ** -0.5)
NEGM = -30000.0


class TT:
    __slots__ = ("name", "w", "r")

    def __init__(self, name):
        self.name = name
        self.w = None
        self.r = {}


class Sched:
    ENGS = ("pe", "act", "dve", "pool", "sp")

    def __init__(self, nc, n_dma_sems=12):
        self.nc = nc
        self.ops = {e: [] for e in self.ENGS}
        self.cnt = {e: 0 for e in self.ENGS}
        self.seen = {e: {} for e in self.ENGS}
        self.n_dma = n_dma_sems
        self.dma_val = {}
        self.dma_rr = {"sp": 0, "pool": 0, "act": 0}
        self.sems = {}
        self.final_waits = []

    def _need(self, eng, deps, key, val):
        if self.seen[eng].get(key, 0) >= val:
            return
        deps[key] = max(deps.get(key, 0), val)

    def _deps(self, eng, reads, writes, is_dma=False):
        deps = {}
        for t in reads:
            if t.w is not None:
                self._need(eng, deps, t.w[0], t.w[1])
        for t in writes:
            if t.w is not None and (is_dma or t.w[0] != eng):
                self._need(eng, deps, t.w[0], t.w[1])
            for k, v in t.r.items():
                if is_dma or k != eng:
                    self._need(eng, deps, k, v)
        for k, v in deps.items():
            self.seen[eng][k] = v
        return list(deps.items())

    def op(self, eng, fn, reads=(), writes=()):
        waits = self._deps(eng, reads, writes)
        self.cnt[eng] += 1
        c = self.cnt[eng]
        self.ops[eng].append((waits, fn, (eng, 1)))
        for t in reads:
            t.r[eng] = c
        for t in writes:
            t.w = (eng, c)
            t.r = {}
        return c

    def dma(self, q, fn, reads=(), writes=()):
        s = self.dma_rr[q]
        self.dma_rr[q] = (s + 1) % self.n_dma
        key = "d%s%d" % (q, s)
        s = key
        waits = self._deps(q, reads, writes, is_dma=True)
        prev = self.dma_val.get(s, 0)
        if prev > 0 and self.seen[q].get(key, 0) < prev:
            waits.append((key, prev))
            self.seen[q][key] = prev
        self.dma_val[s] = prev + 16
        v = self.dma_val[s]
        self.ops[q].append((waits, fn, (key, 16)))
        for t in reads:
            t.r[key] = v
        for t in writes:
            t.w = (key, v)
            t.r = {}

    def finish(self, eng, tiles):
        waits = [t.w for t in tiles if t.w is not None]
        self.final_waits.append((eng, waits))

    def emit(self, stack):
        nc = self.nc
        keys = list(self.ENGS) + sorted(self.dma_val.keys())
        for k in keys:
            self.sems[k] = stack.enter_context(nc.semaphore("s_" + k))
        block = stack.enter_context(nc.Block())
        sems = self.sems

        def run(eng_name):
            def body(e):
                for waits, fn, inc in self.ops[eng_name]:
                    for k, v in waits:
                        e.wait_ge(sems[k], v)
                    ins = fn(e)
                    ins.then_inc(sems[inc[0]], inc[1])
                for en, waits in self.final_waits:
                    if en == eng_name:
                        for k, v in waits:
                            e.wait_ge(sems[k], v)
            return body

        block.tensor(run("pe"))
        block.scalar(run("act"))
        block.vector(run("dve"))
        block.gpsimd(run("pool"))
        block.sync(run("sp"))


def build_program(stage=99):
    nc = bass.Bass("TRN2", target_bir_lowering=False, dynamic_dma_scratch_size=2048)
    dt = nc.dram_tensor
    x_d = dt("x", [SEQ, D], F32, kind="ExternalInput").ap()
    meta_d = dt("meta", [16, D], F32, kind="ExternalInput").ap()
    ws_d = dt("ws", [NSLOT, 128, 4096], F32, kind="ExternalInput").ap()
    wd_d = dt("wd", [2, 128, NJ * 1024], F32, kind="ExternalInput").ap()
    gcol_d = dt("gcol", [128, 24], F32, kind="ExternalInput").ap()
    gfin_d = dt("gfin", [D], F32, kind="ExternalInput").ap()
    sink_d = dt("sink", [8], F32, kind="ExternalInput").ap()
    cs_d = dt("cs", [NBLK * 128, 64], F32, kind="ExternalInput").ap()
    cst_d = dt("cst", [128, 1152], F32, kind="ExternalInput").ap()
    p2_d = dt("p2", [128, 32], F32, kind="ExternalInput").ap()
    wsb_d = dt("wsb", [NSLOT, 128, 4096], BF16, kind="Internal").ap()
    wdb_d = dt("wdb", [2, 128, NJ * 1024], BF16, kind="Internal").ap()
    out_d = dt("out", [SEQ, D], F32, kind="ExternalOutput").ap()

    with ExitStack() as st:
        def sb(n, s, d):
            return st.enter_context(nc.sbuf_tensor(n, s, d))

        def ps(n, s, d):
            return st.enter_context(nc.psum_tensor(n, s, d))

        PA = ps("PA", [128, 4, 512], F32)
        PB = ps("PB", [128, 2, 512], F32)
        PTR = ps("PTR", [128, 2, 1024], BF16)

        X = sb("X", [128, 4, 1024], F32)
        XT = sb("XT", [128, 8, 512], BF16)
        BIG = sb("BIG", [128, 7168], F32)
        AT = BIG[:].bitcast(BF16)
        SG = sb("SG", [128, 2, 512], BF16)
        WR = sb("WR", [128, 3, 4096], BF16)
        WD = sb("WD", [128, NJ * 1024], BF16)
        SGA = sb("SGA", [128, 4, 1024], BF16)
        SGB = sb("SGB", [128, 4, 1024], BF16)
        ROT = sb("ROT", [128, 28, 64], BF16)
        QT = sb("QT", [128, 14, 128], BF16)
        KST = sb("KST", [128, 2, 2, 128], BF16)
        KAT = sb("KAT", [128, NBLK * 128], BF16)
        KIT = sb("KIT", [128, NBLK * 128], BF16)
        VA = sb("VA", [128, NBLK, 66], BF16)
        VS = sb("VS", [128, 5, 2, 66], BF16)
        SC = sb("SC", [128, NBLK * 128], F32)
        NEG = sb("NEG", [128, NBLK * 128], BF16)
        RL = sb("RL", [128, 4, 512], F32)
        PT = sb("PT", [128, 2, 1024], BF16)
        OSP = sb("OSP", [128, 512], BF16)
        OWP = sb("OWP", [128, 512], BF16)
        OT = sb("OT", [128, 8, 128], BF16)
        MG = sb("MG", [128, 1024], BF16)
        CAUSF = sb("CAUSF", [128, 128], F32)
        CB = sb("CB", [128, 1152], BF16)
        GFIN = sb("GFIN", [128, 1024], F32)
        CS = sb("CS", [128, 4, 64], F32)
        G = sb("G", [128, 24], F32)
        P2 = sb("P2", [128, 32], F32)
        SM = sb("SM", [128, 64], F32)
        WT = sb("WT", [128, 32], F32)
        WI = sb("WI", [128, 4, 4], F32)
        REC = sb("REC", [128, 8], F32)
        ESINK = sb("ESINK", [128, 8], F32)

        SS = SM[:, 0:4]
        MS = SM[:, 4:8]
        RSTD = SM[:, 8:12]
        MHALF = SM[:, 12:16]
        AMAX = SM[:, 16:17]
        LO = SM[:, 17:18]
        RNG = SM[:, 18:19]
        MID = SM[:, 19:20]
        CNT = SM[:, 20:21]
        TV = SM[:, 21:22]
        THR = SM[:, 22:23]
        DEN = SM[:, 24:32]
        SA = SM[:, 32:33]

        NEG1 = WD[:, 0:4224]
        QT1 = WD[:, 4224:6016].rearrange("p (g t) -> p g t", g=14)
        KSTW = WD[:, 6016:7040].rearrange("p (a k t) -> p a k t", a=4, k=2)
        QT2 = WD[:, 7040:8832].rearrange("p (g t) -> p g t", g=14)
        QTs = [QT, QT1, QT2]
        OSPW = WD[:, 8832:10880].rearrange("p (i c) -> p i c", i=4)
        OWPW = WD[:, 10880:12928].rearrange("p (i c) -> p i c", i=4)
        OT2 = WD[:, 12928:13952].rearrange("p (k t) -> p k t", k=8)
        MG2 = WD[:, 13952:14976]
        OTs = [OT[:], OT2]
        MGs = [MG[:], MG2]
        NEGs = [NEG, NEG1]
        S = Sched(nc)
        T = TT
        tX = [T("X%d" % i) for i in range(4)]
        tXT = T("XT")
        tATc = [T("AT%d" % j) for j in range(NJ)]
        tR = [T("R%d" % i) for i in range(4)]
        tSG = [T("SG0"), T("SG1")]
        tWR = [T("WR%d" % i) for i in range(3)]
        tWD = T("WD")
        tSGA = [T("SGA%d" % i) for i in range(4)]
        tSGB = [T("SGB%d" % i) for i in range(4)]
        tROT = T("ROT")
        tQTs = [T("QT0"), T("QT1"), T("QT2")]
        tNEGs = [T("NEG0"), T("NEG1")]
        tNEGAs = [T("NEGA0"), T("NEGA1")]
        tKSTW = [T("KSTW%d" % i) for i in range(4)]
        tOSPs = [T("OSP%d" % i) for i in range(4)]
        tOWPs = [T("OWP%d" % i) for i in range(4)]
        tOTs = [T("OT0"), T("OT1")]
        tMGs = [T("MG0"), T("MG1")]
        tKST0 = T("KST0")
        tKAT, tKIT, tVA = T("KAT"), T("KIT"), T("VA")
        tVS = [T("VS%d" % i) for i in range(5)]
        tSC = T("SC")
        tSA, tMID = T("SA"), T("MID")
        tRL = [T("RL%d" % i) for i in range(4)]
        tPT = [T("PT0"), T("PT1")]
        tOSP, tOWP, tOT, tMG = T("OSP"), T("OWP"), T("OT"), T("MG")
        tC = T("CONST")
        tCS = T("CS")
        tSM = T("SM")
        tBI = T("BI")
        tWI = T("WI")
        tREC = T("REC")
        tPA = [T("PA%d" % i) for i in range(4)]
        tPB = [T("PB0"), T("PB1")]
        tPTR = [T("PTR0"), T("PTR1")]
        tWSB = [T("wsb%d" % i) for i in range(NSLOT)]
        tWDB = [T("wdb0"), T("wdb1")]
        tOUT = [T("out%d" % i) for i in range(40)]
        tF = [T("F%d" % i) for i in range(4)]

        CF = RL[:].rearrange("p a b -> p (a b)")[:, 0:1152]
        S.dma("sp", lambda e: e.dma_start(out=CF, in_=cst_d), writes=[tC] + tRL)
        S.dma("sp", lambda e: e.dma_start(out=CAUSF[:], in_=cst_d[:, 512:640]), writes=[tC])
        S.dma("sp", lambda e: e.dma_start(out=G[:], in_=gcol_d), writes=[tC])
        S.dma("sp", lambda e: e.dma_start(out=P2[:], in_=p2_d), writes=[tC])
        S.dma("sp", lambda e: e.dma_start(out=GFIN[:], in_=gfin_d.partition_broadcast(128)), writes=[tC])
        S.dma("sp", lambda e: e.dma_start(out=ESINK[:], in_=sink_d.partition_broadcast(128)), writes=[tC])
        S.op("dve", lambda e: e.tensor_copy(out=CB[:], in_=CF), reads=[tC] + tRL, writes=[tC])
        S.op("act", lambda e: e.activation(out=ESINK[:], in_=ESINK[:], func=AF.Exp), reads=[tC], writes=[tC])
        S.op("pool", lambda e: e.memset(SM[:, 12:16], -0.5), writes=[tSM])
        S.op("pool", lambda e: e.memset(VA[:, :, 64:66], 1.0), writes=[tVA])
        S.op("pool", lambda e: e.memset(VS[:, :, :, 64:66], 1.0), writes=tVS)
        ID4 = CB[:, 0:512]
        CAUS = CAUSF[:]
        SWCUR = CB[:, 640:768]
        SWPREV = CB[:, 768:896]
        SWPREV1 = CB[:, 896:1024]

        def convert_group(conv_order, first_reads=()):
          fr = list(first_reads)
          for it in conv_order:
            if isinstance(it, str):
                f = int(it[2])
                for hlf in range(2):
                    sl = slice(hlf * 11 * 1024, (hlf + 1) * 11 * 1024)
                    S.dma("pool", lambda e, f=f, sl=sl: e.dma_start(out=wdb_d[f, :, sl], in_=wd_d[f, :, sl], max_dma_last_dim=8192), writes=[tWDB[f]])
            else:
                S.dma("pool", lambda e, it=it: e.dma_start(out=wsb_d[it], in_=ws_d[it], max_dma_last_dim=8192), reads=fr, writes=[tWSB[it]])
                fr = []

        tiles = [[0]] + [list(range(1 + 4 * t, 5 + 4 * t)) for t in range(8)]
        seq = []
        for ti, blks in enumerate(tiles):
            if ti == 0:
                seq += list(range(0, 15))
            else:
                seq += list(range(0, NSLOT))
        ring = {"issued": 0, "used": 0, "holds": set()}

        def issue_next():
            k = ring["issued"]
            if k >= len(seq):
                return
            pos = k % 3
            slot = seq[k]
            S.dma("sp", lambda e, pos=pos, slot=slot: e.dma_start(out=WR[:, pos, :], in_=wsb_d[slot]),
                  reads=[tWSB[slot]], writes=[tWR[pos]])
            ring["issued"] += 1

        def pump():
            k = ring["used"] - 1
            released = min([k] + list(ring["holds"]))
            while ring["issued"] < min(k + 3, released + 3, len(seq)):
                issue_next()

        def use_slot(expect, hold=False):
            k = ring["used"]
            assert seq[k] == expect, (k, seq[k], expect)
            ring["used"] += 1
            if hold:
                ring["holds"].add(k)
            pump()
            assert ring["issued"] > k
            return k % 3

        def unhold_all():
            ring["holds"].clear()
            pump()

        def load_wd(f, alias=False):
            wr = [tWD] + ((tQTs + tNEGs + tNEGAs + tKSTW + tOSPs + tOWPs + [tOTs[1], tMGs[1]]) if alias else [])
            S.dma("pool", lambda e, f=f: e.dma_start(out=WD[:], in_=wdb_d[f]), reads=[tWDB[f]], writes=wr)

        SGf = SG[:].rearrange("p a b -> p (a b)")
        XSs = [PT[:, 0, :], PT[:, 1, :]]

        def norm_to_xt(NB, gi, ptr_par):
            for i in range(NB):
                S.op("act", lambda e, i=i: e.activation(out=SGf, in_=X[:, i, :], func=AF.Square, accum_out=SS[:, i:i + 1]),
                     reads=[tX[i]], writes=[tSG[0], tSG[1], tSM])
            S.op("dve", lambda e: e.tensor_scalar(out=MS[:, 0:NB], in0=SS[:, 0:NB], scalar1=1.0 / D, scalar2=EPS, op0=ALU.mult, op1=ALU.add),
                 reads=[tSM], writes=[tSM])
            S.op("pool", lambda e: e.tensor_tensor(out=RSTD[:, 0:NB], in0=MS[:, 0:NB], in1=MHALF[:, 0:NB], op=ALU.pow),
                 reads=[tSM], writes=[tSM])
            for i in range(NB):
                xs, txs = XSs[i % 2], tPT[i % 2]
                S.op("act", lambda e, i=i, xs=xs: e.mul(out=xs, in_=X[:, i, :], mul=RSTD[:, i:i + 1]), reads=[tX[i], tSM], writes=[txs])
                par = (ptr_par + i) % 2

                def tr(e, par=par, xs=xs):
                    ins = None
                    for kc in range(8):
                        ins = e.transpose(out=PTR[:, par, kc * 128:(kc + 1) * 128], in_=xs[:, kc * 128:(kc + 1) * 128], identity=CB[:, 0:128])
                    return ins
                S.op("pe", tr, reads=[txs, tC], writes=[tPTR[par]])
                S.op("dve", lambda e, i=i, par=par: e.tensor_tensor(
                    out=XT[:, :, i * 128:(i + 1) * 128],
                    in0=PTR[:, par, :].rearrange("p (k t) -> p k t", k=8),
                    in1=G[:, gi * 8:(gi + 1) * 8].unsqueeze(2).to_broadcast([128, 8, 128]), op=ALU.mult),
                    reads=[tPTR[par], tC], writes=[tXT])

        def ffn(f, NB, alias_tiles):
            TK = NB * 128
            base = 0 if f == 0 else 23
            first = True
            for j in range(NJ):
                if j % 2 == 0:
                    pos = use_slot(base + j // 2)
                cj = j % 2
                gp = j % 2
                woff = cj * 2048

                def gu(e, pos=pos, woff=woff, gp=gp):
                    ins = None
                    for g in range(2):
                        for kc in range(8):
                            o = woff + g * 1024 + kc * 128
                            ins = e.matmul(PA[:, g * 2 + gp, 0:TK], lhsT=WR[:, pos, o:o + 128], rhs=XT[:, kc, 0:TK], start=(kc == 0), stop=(kc == 7))
                    return ins
                S.op("pe", gu, reads=[tWR[pos], tXT], writes=[tPA[gp], tPA[2 + gp]])
                S.op("act", lambda e, gp=gp: e.activation(out=SG[:, gp, 0:TK], in_=PA[:, gp, 0:TK], func=AF.Silu), reads=[tPA[gp]], writes=[tSG[gp]])
                wr = [tATc[j]] + (alias_tiles if first else [])
                first = False
                S.op("dve", lambda e, j=j, gp=gp: e.tensor_tensor(out=AT[:, j * 512:j * 512 + TK], in0=PA[:, 2 + gp, 0:TK], in1=SG[:, gp, 0:TK], op=ALU.mult),
                     reads=[tPA[2 + gp], tSG[gp]], writes=wr)
            n = 0
            for i in range(NB):
                for half in range(2):
                    par = n % 2
                    n += 1

                    def dn(e, i=i, half=half, par=par):
                        ins = None
                        for j in range(NJ):
                            ins = e.matmul(PB[:, par, :], lhsT=AT[:, j * 512 + i * 128:j * 512 + (i + 1) * 128],
                                           rhs=WD[:, j * 1024 + half * 512:j * 1024 + (half + 1) * 512], start=(j == 0), stop=(j == NJ - 1))
                        return ins
                    S.op("pe", dn, reads=tATc + [tWD], writes=[tPB[par]])
                    S.op("dve", lambda e, i=i, half=half, par=par: e.scalar_tensor_tensor(
                        out=X[:, i, half * 512:(half + 1) * 512], in0=PB[:, par, :], scalar=0.5, in1=X[:, i, half * 512:(half + 1) * 512],
                        op0=ALU.mult, op1=ALU.add), reads=[tPB[par], tX[i]], writes=[tX[i]])

        Rv = BIG[:].rearrange("p (i c) -> p i c", i=4)

        def project(NB, blks, chunks):
            n = 0
            for cc in chunks:
                pos = use_slot(11 + cc)
                for i in range(NB):
                    par = n % 2
                    n += 1
                    c = blks[i]

                    def pj(e, pos=pos, i=i, par=par):
                        ins = None
                        for kc in range(8):
                            ins = e.matmul(PB[:, par, :], lhsT=XT[:, kc, i * 128:(i + 1) * 128], rhs=WR[:, pos, kc * 512:(kc + 1) * 512], start=(kc == 0), stop=(kc == 7))
                        return ins
                    S.op("pe", pj, reads=[tWR[pos], tXT], writes=[tPB[par]])
                    if cc < 3:
                        wr = [tR[i]] + (tATc if cc == 0 and i == 0 else [])
                        eng = "act" if (n % 2 == 0) else "dve"
                        if eng == "act":
                            S.op("act", lambda e, i=i, cc=cc, par=par: e.copy(out=Rv[:, i, cc * 512:(cc + 1) * 512], in_=PB[:, par, :]), reads=[tPB[par]], writes=wr)
                        else:
                            S.op("dve", lambda e, i=i, cc=cc, par=par: e.tensor_copy(out=Rv[:, i, cc * 512:(cc + 1) * 512], in_=PB[:, par, :]), reads=[tPB[par]], writes=wr)
                    elif cc == 3:
                        S.op("act", lambda e, i=i, par=par: e.copy(out=Rv[:, i, 1536:1792], in_=PB[:, par, 0:256]), reads=[tPB[par]], writes=[tR[i]])
                        S.op("act", lambda e, c=c, par=par: e.copy(out=VA[:, c, 0:64], in_=PB[:, par, 256:320]), reads=[tPB[par]], writes=[tVA])
                        S.op("act", lambda e, i=i, par=par: e.copy(out=VS[:, i + 1, :, 0:64], in_=PB[:, par, 320:448].rearrange("p (k d) -> p k d", k=2)),
                             reads=[tPB[par]], writes=[tVS[i + 1]])
                        S.op("act", lambda e, i=i, par=par: e.mul(out=WI[:, i, :], in_=PB[:, par, 448:452], mul=IDX_SCALE),
                             reads=[tPB[par]], writes=[tWI])
                    else:
                        gsel = (cc - 4) // 2
                        hh = (cc - 4) % 2
                        dst = SGA if gsel == 0 else SGB
                        tdst = tSGA if gsel == 0 else tSGB
                        S.op("act", lambda e, dst=dst, i=i, hh=hh, par=par: e.activation(out=dst[:, i, hh * 512:(hh + 1) * 512], in_=PB[:, par, :], func=AF.Sigmoid),
                             reads=[tPB[par]], writes=[tdst[i]])
                    yield

        T1 = RL[:, 0:2, :].rearrange("p a b -> p (a b)")[:, 0:896].rearrange("p (h d) -> p h d", h=28)
        T2 = RL[:, 2:4, :].rearrange("p a b -> p (a b)")[:, 0:896].rearrange("p (h d) -> p h d", h=28)

        def rope_block(i, c, tile0=False):
            QTx, tQ = QTs[i % 3], tQTs[i % 3]
            Rb = Rv[:, i, :].rearrange("p (h t d) -> p h t d", h=28, t=2)
            x1 = Rb[:, :, 0, :]
            x2 = Rb[:, :, 1, :]
            cos = CS[:, i, 0:32].unsqueeze(1).to_broadcast([128, 28, 32])
            sin = CS[:, i, 32:64].unsqueeze(1).to_broadcast([128, 28, 32])
            rl01, rl23 = [tRL[0], tRL[1]], [tRL[2], tRL[3]]
            P = "pool"
            S.op(P, lambda e: e.tensor_tensor(out=T1, in0=x1, in1=cos, op=ALU.mult), reads=[tR[i], tCS], writes=rl01)
            S.op(P, lambda e: e.tensor_tensor(out=T2, in0=x2, in1=sin, op=ALU.mult), reads=[tR[i], tCS], writes=rl23)
            S.op(P, lambda e: e.tensor_tensor(out=ROT[:, :, 0:32], in0=T1, in1=T2, op=ALU.subtract), reads=rl01 + rl23, writes=[tROT])
            S.op(P, lambda e: e.tensor_tensor(out=T1, in0=x2, in1=cos, op=ALU.mult), reads=[tR[i], tCS], writes=rl01)
            S.op(P, lambda e: e.tensor_tensor(out=T2, in0=x1, in1=sin, op=ALU.mult), reads=[tR[i], tCS], writes=rl23)
            S.op(P, lambda e: e.tensor_tensor(out=ROT[:, :, 32:64], in0=T1, in1=T2, op=ALU.add), reads=rl01 + rl23, writes=[tROT])
            ROTf = ROT[:].rearrange("p h d -> p (h d)")

            def tr(e):
                ins = None
                for g in range(14):
                    ins = e.transpose(out=PTR[:, g // 8, (g % 8) * 128:(g % 8 + 1) * 128], in_=ROTf[:, g * 128:(g + 1) * 128], identity=CB[:, 0:128])
                return ins
            S.op("pe", tr, reads=[tROT, tC], writes=[tPTR[0], tPTR[1]])
            S.op("act", lambda e: e.copy(out=QTx[:, 0:8, :], in_=PTR[:, 0, :].rearrange("p (g t) -> p g t", g=8)), reads=[tPTR[0]], writes=[tQ])
            S.op("dve", lambda e: e.tensor_copy(out=QTx[:, 8:14, :], in_=PTR[:, 1, 0:768].rearrange("p (g t) -> p g t", g=6)), reads=[tPTR[1]], writes=[tQ])
            S.op("pool", lambda e: e.tensor_copy(out=KAT[:, c * 128:(c + 1) * 128], in_=QTx[:, 6, :]), reads=[tQ], writes=[tKAT])
            S.op("pool", lambda e: e.tensor_copy(out=KIT[:, c * 128:(c + 1) * 128], in_=QTx[:, 7, :]), reads=[tQ], writes=[tKIT])
            if tile0 or i == 3:
                S.op("pool", lambda e: e.tensor_copy(out=KST[:, 0, :, :], in_=QTx[:, 12:14, :]), reads=[tQ], writes=[tKST0])
            if not tile0:
                S.op("pool", lambda e: e.tensor_copy(out=KSTW[:, i, :, :], in_=QTx[:, 12:14, :]), reads=[tQ], writes=[tKSTW[i]])

        def attn_core(keyblocks, out_sb, t_out, sink, QTx, tQ):
            nk = len(keyblocks)

            def emit_sc(jj):
                s = jj % 2
                kfn, negap, vfn, rds, swa = keyblocks[jj]

                def sc(e):
                    ins = None
                    for hf in range(2):
                        if not swa:
                            e.matmul(PA[:, 2 * s + hf, :], lhsT=kfn(hf, 0), rhs=QTx[hf * 64:(hf + 1) * 64, 0:4, :], start=True, stop=False)
                        else:
                            for kap in range(2):
                                e.matmul(PA[:, 2 * s + hf, kap * 256:(kap + 1) * 256], lhsT=kfn(hf, kap),
                                         rhs=QTx[hf * 64:(hf + 1) * 64, 8 + 2 * kap:10 + 2 * kap, :], start=(kap == 0), stop=False, skip_group_check=True)
                    for hf in range(2):
                        ins = e.matmul(PA[:, 2 * s + hf, :], lhsT=negap, rhs=ID4, start=False, stop=True, skip_group_check=True)
                    return ins
                S.op("pe", sc, reads=rds + [tQ, tC], writes=[tPA[2 * s], tPA[2 * s + 1]])

            def emit_exp_pv(jj):
                s = jj % 2
                kfn, negap, vfn, rds, swa = keyblocks[jj]
                S.op("act", lambda e: e.activation(out=PT[:, s, :], in_=PA[:, 2 * s:2 * s + 2, :].rearrange("p a b -> p (a b)"), func=AF.Exp, scale=0.125),
                     reads=[tPA[2 * s], tPA[2 * s + 1]], writes=[tPT[s]])

                def pv(e):
                    ins = None
                    for hf in range(2):
                        for pr in range(4):
                            ins = e.matmul(PB[:, hf, pr * 65:(pr + 1) * 65], lhsT=PT[:, s, (hf * 4 + pr) * 128:(hf * 4 + pr + 1) * 128], rhs=vfn(pr),
                                           start=(jj == 0 and pr == 0), stop=(jj == nk - 1 and pr == 3), skip_group_check=True)
                    return ins
                S.op("pe", pv, reads=rds + [tPT[s]], writes=[tPB[0], tPB[1]])

            emit_sc(0)
            for jj in range(nk):
                if jj + 1 < nk:
                    emit_sc(jj + 1)
                emit_exp_pv(jj)
                yield
            PBv = PB[:, :, 0:260].rearrange("p b (r e) -> p b r e", e=65)
            RECv = REC[:].rearrange("p (b r o) -> p b r o", b=2, o=1)
            if sink:
                S.op("dve", lambda e: e.tensor_tensor(out=RECv, in0=PBv[:, :, :, 64:65], in1=ESINK[:].rearrange("p (b r o) -> p b r o", b=2, o=1), op=ALU.add),
                     reads=[tPB[0], tPB[1], tC], writes=[tREC])
                S.op("dve", lambda e: e.reciprocal(out=REC[:], in_=REC[:]), reads=[tREC], writes=[tREC])
            else:
                S.op("dve", lambda e: e.reciprocal(out=RECv, in_=PBv[:, :, :, 64:65]), reads=[tPB[0], tPB[1]], writes=[tREC])
            S.op("dve", lambda e: e.tensor_tensor(out=out_sb[:].rearrange("p (b r d) -> p b r d", b=2, r=4), in0=PBv[:, :, :, 0:64],
                                                  in1=RECv.to_broadcast([128, 2, 4, 64]), op=ALU.mult),
                 reads=[tPB[0], tPB[1], tREC], writes=[t_out])

        def index_bisect(i, c, frac=0.42):
            split = frac < 1.0
            QTx, tQ = QTs[i % 3], tQTs[i % 3]
            NEGx, tNEG, tNEGA = NEGs[i % 2], tNEGs[i % 2], tNEGAs[i % 2]
            n = (c + 1) * 128
            nch = (n + 511) // 512
            for ch in range(nch):
                w = min(512, n - ch * 512)

                for pr in range(2):
                    def ix(e, ch=ch, w=w, pr=pr):
                        ins = None
                        for hf in range(2):
                            ins = e.matmul(PA[:, 2 * pr + hf, 0:w], lhsT=QTx[hf * 64:(hf + 1) * 64, 4 + pr, :],
                                           rhs=KIT[hf * 64:(hf + 1) * 64, ch * 512:ch * 512 + w], start=True, stop=True)
                        return ins
                    S.op("pe", ix, reads=[tQ, tKIT], writes=[tPA[2 * pr], tPA[2 * pr + 1]])
                for h in range(4):
                    S.op("act", lambda e, h=h, w=w: e.activation(out=RL[:, h, 0:w], in_=PA[:, h, 0:w], func=AF.Relu), reads=[tPA[h]], writes=[tRL[h]])
                S.op("dve", lambda e, ch=ch, w=w: e.tensor_scalar(out=SC[:, ch * 512:ch * 512 + w], in0=RL[:, 0, 0:w], scalar1=WI[:, i, 0:1], scalar2=None, op0=ALU.mult),
                     reads=[tRL[0], tWI], writes=[tSC])
                for h in range(1, 4):
                    S.op("dve", lambda e, ch=ch, w=w, h=h: e.scalar_tensor_tensor(out=SC[:, ch * 512:ch * 512 + w], in0=RL[:, h, 0:w], scalar=WI[:, i, h:h + 1],
                                                                                 in1=SC[:, ch * 512:ch * 512 + w], op0=ALU.mult, op1=ALU.add),
                         reads=[tRL[h], tWI, tSC], writes=[tSC])
            V = "dve"
            S.op(V, lambda e: e.tensor_reduce(out=AMAX, in_=SC[:, 0:n], axis=AX.X, op=ALU.max, apply_absolute_value=True), reads=[tSC], writes=[tBI])
            S.op(V, lambda e: e.memset(SC[:, 0:112], -1e30), reads=[tSC], writes=[tSC])
            S.op(V, lambda e: e.tensor_tensor(out=SC[:, c * 128:(c + 1) * 128], in0=SC[:, c * 128:(c + 1) * 128], in1=CAUS, op=ALU.add), reads=[tSC, tC], writes=[tSC])
            S.op(V, lambda e: e.tensor_scalar(out=LO, in0=AMAX, scalar1=-1.001, scalar2=-1e-20, op0=ALU.mult, op1=ALU.add), reads=[tBI], writes=[tBI])
            S.op(V, lambda e: e.tensor_tensor(out=RNG, in0=AMAX, in1=LO, op=ALU.subtract), reads=[tBI], writes=[tBI])
            S.op(V, lambda e: e.tensor_scalar(out=WT[:, 0:NIT + 1], in0=P2[:, 0:NIT + 1], scalar1=RNG, scalar2=None, op0=ALU.mult), reads=[tBI, tC], writes=[tBI])
            S.op(V, lambda e: e.tensor_tensor(out=MID, in0=LO, in1=WT[:, 0:1], op=ALU.add), reads=[tBI], writes=[tMID])
            nd = (min(c, max(1, int(frac * (c + 1) + 0.5))) * 128) if split else n
            na = n - nd
            for it in range(NIT):
                if split:
                    S.op("act", lambda e: e.activation(out=NEGx[:, nd:n], in_=SC[:, nd:n], func=AF.Sign, bias=MID, scale=-1.0, accum_out=SA),
                         reads=[tSC, tMID], writes=[tNEGA, tSA])
                S.op(V, lambda e: e.tensor_scalar(out=NEGx[:, 0:nd], in0=SC[:, 0:nd], scalar1=MID, scalar2=None, op0=ALU.is_gt, op1=ALU.add, accum_out=CNT),
                     reads=[tSC, tMID], writes=[tNEG, tBI])
                if split:
                    S.op(V, lambda e: e.scalar_tensor_tensor(out=CNT, in0=SA, scalar=-0.5, in1=CNT, op0=ALU.mult, op1=ALU.add), reads=[tSA, tBI], writes=[tBI])
                S.op(V, lambda e: e.tensor_scalar(out=TV, in0=CNT, scalar1=255.5 - na / 2.0, scalar2=0.5, op0=ALU.is_gt, op1=ALU.subtract), reads=[tBI], writes=[tBI])
                S.op(V, lambda e, it=it: e.scalar_tensor_tensor(out=MID, in0=TV, scalar=WT[:, it:it + 1], in1=MID, op0=ALU.mult, op1=ALU.add), reads=[tBI, tMID], writes=[tMID])
                yield
            S.op(V, lambda e: e.tensor_tensor(out=THR, in0=MID, in1=WT[:, NIT:NIT + 1], op=ALU.subtract), reads=[tBI, tMID], writes=[tBI])
            S.op(V, lambda e: e.tensor_scalar(out=NEGx[:, 0:n], in0=SC[:, 0:n], scalar1=THR, scalar2=NEGM, op0=ALU.is_le, op1=ALU.mult), reads=[tSC, tBI], writes=[tNEG, tNEGA])

        def attn_main(i, c):
            QTx, tQ = QTs[i % 3], tQTs[i % 3]
            NEGx, tNEG, tNEGA = NEGs[i % 2], tNEGs[i % 2], tNEGAs[i % 2]
            kbs = []
            for j in range(c + 1):
                kbs.append((lambda hf, kap, j=j: KAT[hf * 64:(hf + 1) * 64, j * 128:(j + 1) * 128], NEGx[:, j * 128:(j + 1) * 128],
                            lambda pr, j=j: VA[:, j, 0:65], [tKAT, tNEG, tNEGA, tVA], False))
            yield from attn_core(kbs, OSPW[:, i, :], tOSPs[i], False, QTx, tQ)
            if i == 0:
                kprev = lambda hf, kap: KST[hf * 64:(hf + 1) * 64, 0, kap, :]
                tkp = tKST0
            else:
                kprev = lambda hf, kap: KSTW[hf * 64:(hf + 1) * 64, i - 1, kap, :]
                tkp = tKSTW[i - 1]
            kbs = [
                (kprev, SWPREV1 if c == 1 else SWPREV, lambda pr: VS[:, i, pr // 2, 0:65], [tkp, tVS[i], tC], True),
                (lambda hf, kap: KSTW[hf * 64:(hf + 1) * 64, i, kap, :], SWCUR, lambda pr: VS[:, i + 1, pr // 2, 0:65], [tKSTW[i], tVS[i + 1], tC], True),
            ]
            yield from attn_core(kbs, OWPW[:, i, :], tOWPs[i], True, QTx, tQ)

        def tail_a(i):
            par = i % 2
            OTx, tOTx = OTs[i % 2], tOTs[i % 2]

            def tro(e):
                ins = None
                for k in range(4):
                    e.transpose(out=PTR[:, par, k * 128:(k + 1) * 128], in_=OSPW[:, i, k * 128:(k + 1) * 128], identity=CB[:, 0:128])
                for k in range(4):
                    ins = e.transpose(out=PTR[:, par, (4 + k) * 128:(5 + k) * 128], in_=OWPW[:, i, k * 128:(k + 1) * 128], identity=CB[:, 0:128])
                return ins
            S.op("pe", tro, reads=[tOSPs[i], tOWPs[i], tC], writes=[tPTR[par]])
            S.op("act", lambda e: e.copy(out=OTx, in_=PTR[:, par, :].rearrange("p (k t) -> p k t", k=8)), reads=[tPTR[par]], writes=[tOTx])

        def tail_b(i, pos_bs, pos_bw):
            OTx, tOTx = OTs[i % 2], tOTs[i % 2]
            MGx, tMGx = MGs[i % 2], tMGs[i % 2]

            def br(e):
                ins = None
                for b, pos in enumerate((pos_bs, pos_bw)):
                    for half in range(2):
                        for kc in range(4):
                            ins = e.matmul(PA[:, 2 * b + half, :], lhsT=OTx[:, 4 * b + kc, :], rhs=WR[:, pos, kc * 1024 + half * 512:kc * 1024 + (half + 1) * 512],
                                           start=(kc == 0), stop=(kc == 3))
                return ins
            S.op("pe", br, reads=[tOTx, tWR[pos_bs], tWR[pos_bw]], writes=tPA)
            M1 = RL[:, 0:2, :]
            M2 = RL[:, 2:4, :]
            S.op("dve", lambda e: e.tensor_tensor(out=M1, in0=PA[:, 0:2, :], in1=SGA[:, i, :].rearrange("p (a b) -> p a b", a=2), op=ALU.mult),
                 reads=[tPA[0], tPA[1], tSGA[i]], writes=[tRL[0], tRL[1]])
            S.op("dve", lambda e: e.tensor_tensor(out=M2, in0=PA[:, 2:4, :], in1=SGB[:, i, :].rearrange("p (a b) -> p a b", a=2), op=ALU.mult),
                 reads=[tPA[2], tPA[3], tSGB[i]], writes=[tRL[2], tRL[3]])
            S.op("pool", lambda e: e.tensor_tensor(out=MGx.rearrange("p (a b) -> p a b", a=2), in0=M1, in1=M2, op=ALU.add), reads=tRL, writes=[tMGx])

        def tail_c(i):
            MGx, tMGx = MGs[i % 2], tMGs[i % 2]
            par2 = (i + 1) % 2

            def trm(e):
                ins = None
                for k in range(8):
                    ins = e.transpose(out=PTR[:, par2, k * 128:(k + 1) * 128], in_=MGx[:, k * 128:(k + 1) * 128], identity=CB[:, 0:128])
                return ins
            S.op("pe", trm, reads=[tMGx, tC], writes=[tPTR[par2]])
            S.op("act", lambda e: e.copy(out=XT[:, :, i * 128:(i + 1) * 128], in_=PTR[:, par2, :].rearrange("p (k t) -> p k t", k=8)), reads=[tPTR[par2]], writes=[tXT])

        def out_proj(NB, pos_a, pos_b):
            n = 0
            for i in range(NB):
                for half in range(2):
                    par = n % 2
                    n += 1

                    def wo(e, i=i, half=half, par=par):
                        ins = None
                        for kc in range(8):
                            pos = pos_a if kc < 4 else pos_b
                            o = (kc % 4) * 1024 + half * 512
                            ins = e.matmul(PB[:, par, :], lhsT=XT[:, kc, i * 128:(i + 1) * 128], rhs=WR[:, pos, o:o + 512], start=(kc == 0), stop=(kc == 7))
                        return ins
                    S.op("pe", wo, reads=[tXT, tWR[pos_a], tWR[pos_b]], writes=[tPB[par]])
                    S.op("dve", lambda e, i=i, half=half, par=par: e.tensor_tensor(out=X[:, i, half * 512:(half + 1) * 512], in0=PB[:, par, :],
                                                                                   in1=X[:, i, half * 512:(half + 1) * 512], op=ALU.add),
                         reads=[tPB[par], tX[i]], writes=[tX[i]])

        def final_norm_store(NB, blks, ti):
            for i in range(NB):
                tf = tF[i]
                S.op("act", lambda e, i=i: e.activation(out=SGf, in_=X[:, i, :], func=AF.Square, accum_out=SM[:, 40 + i:41 + i]), reads=[tX[i]], writes=[tSG[0], tSG[1], tf])
                S.op("dve", lambda e, i=i: e.tensor_scalar(out=SM[:, 44 + i:45 + i], in0=SM[:, 40 + i:41 + i], scalar1=1.0 / D, scalar2=EPS, op0=ALU.mult, op1=ALU.add), reads=[tf], writes=[tf])
                S.op("pool", lambda e, i=i: e.tensor_tensor(out=SM[:, 48 + i:49 + i], in0=SM[:, 44 + i:45 + i], in1=MHALF[:, 0:1], op=ALU.pow), reads=[tf, tSM], writes=[tf])
                S.op("dve", lambda e, i=i: e.scalar_tensor_tensor(out=X[:, i, :], in0=X[:, i, :], scalar=SM[:, 48 + i:49 + i], in1=GFIN[:], op0=ALU.mult, op1=ALU.mult),
                     reads=[tX[i], tf, tC], writes=[tX[i]])
                r0 = (blks[i] - 1) * 128
                S.dma("sp", lambda e, i=i, r0=r0: e.dma_start(out=out_d[r0:r0 + 128, :], in_=X[:, i, :]), reads=[tX[i]], writes=[tOUT[ti * 4 + i]])
                if ti < 8:
                    r1 = r0 + 512
                    S.dma("pool", lambda e, i=i, r1=r1: e.dma_start(out=X[:, i, :], in_=x_d[r1:r1 + 128, :]), writes=[tX[i]])

        for ti, blks in enumerate(tiles):
            NB = len(blks)
            if ti == 0:
                S.op("pool", lambda e: e.memset(X[:, 0, :], 0.0), writes=[tX[0]])
                S.dma("pool", lambda e: e.dma_start(out=X[112:128, 0, :], in_=meta_d), writes=[tX[0]])
            elif ti == 1:
                r0 = (blks[0] - 1) * 128
                S.dma("pool", lambda e, r0=r0: e.dma_start(out=X[:, 0:4, :], in_=x_d[r0:r0 + 512, :].rearrange("(i p) d -> p i d", p=128)), writes=tX)
            c0 = blks[0] * 128
            S.dma("pool", lambda e, c0=c0, NB=NB: e.dma_start(out=CS[:, 0:NB, :], in_=cs_d[c0:c0 + NB * 128, :].rearrange("(i p) d -> p i d", p=128)), writes=[tCS])
            if ti == 0:
                convert_group(list(range(0, 11)) + ["wd0"] + list(range(11, 15)))
                load_wd(0)
            norm_to_xt(NB, 0, 0)
            if ti == 1:
                convert_group(list(range(15, NSLOT)) + ["wd1"], first_reads=[tXT])
            ffn(0, NB, tR)
            norm_to_xt(NB, 1, 0)
            for _ in project(NB, blks, range(0, 4)):
                pass
            if ti == 0:
                S.op("pool", lambda e: e.tensor_copy(out=VS[:, 0, :, 0:64], in_=VS[:, 1, :, 0:64]), reads=[tVS[1]], writes=[tVS[0]])
                rope_block(0, 0, tile0=True)
                continue
            rope_block(0, blks[0])
            gb0 = index_bisect(0, blks[0], frac=0.42)
            next(gb0)
            gp = project(NB, blks, range(4, 8))
            p_done = b_done = False
            while not (p_done and b_done):
                if not p_done:
                    try:
                        next(gp)
                    except StopIteration:
                        p_done = True
                if not b_done:
                    try:
                        next(gb0)
                    except StopIteration:
                        b_done = True
            pos_bs = use_slot(19, hold=True)
            pos_bw = use_slot(20, hold=True)
            rope_block(1, blks[1])
            for i in range(NB):
                ga = attn_main(i, blks[i])
                na_steps = blks[i] + 3
                roped = (i + 2 >= NB)
                if i + 1 < NB:
                    gb = index_bisect(i + 1, blks[i + 1], frac=0.5)
                    next(gb)
                    ca, cb, a_done, b_done = 0, 1, False, False
                    while not (a_done and b_done):
                        if b_done or (not a_done and ca * NIT <= cb * na_steps):
                            try:
                                next(ga)
                                ca += 1
                            except StopIteration:
                                a_done = True
                            if not roped and ca >= na_steps // 2:
                                rope_block(i + 2, blks[i + 2])
                                roped = True
                        else:
                            try:
                                next(gb)
                                cb += 1
                            except StopIteration:
                                b_done = True
                    if not roped:
                        rope_block(i + 2, blks[i + 2])
                else:
                    for _ in ga:
                        pass
            tail_a(0)
            for i in range(NB):
                if i + 1 < NB:
                    tail_a(i + 1)
                tail_b(i, pos_bs, pos_bw)
                if i >= 1:
                    tail_c(i - 1)
            tail_c(NB - 1)
            load_wd(1, alias=True)
            S.op("pool", lambda e: e.tensor_copy(out=VS[:, 0, :, 0:64], in_=VS[:, 4, :, 0:64]), reads=[tVS[4]], writes=[tVS[0]])
            unhold_all()
            pos_a = use_slot(21, hold=True)
            pos_b = use_slot(22, hold=True)
            out_proj(NB, pos_a, pos_b)
            unhold_all()
            norm_to_xt(NB, 2, 0)
            ffn(1, NB, tR)
            if ti < 8:
                load_wd(0)
            final_norm_store(NB, blks, ti)
            if stage == 5 and ti == 1:
                break
        S.finish("sp", tOUT)
        S.emit(st)
    return nc


def _host_layout(inp):
    f32 = np.float32
    w_in = np.asarray(inp["w_in"][0], f32)
    cols = np.arange(3780)
    qa, ka, va = cols[0:512], cols[512:576], cols[576:640]
    qi, ki, wi = cols[640:896], cols[896:960], cols[960:964]
    qs, ksw, vsw = cols[964:1476], cols[1476:1604], cols[1604:1732]
    ga, gb = cols[1732:2756], cols[2756:3780]
    perm = np.concatenate([qa, qi, ka, ka, ki, ki, qs, ksw[:64], ksw[:64], ksw[64:], ksw[64:], va, vsw, wi])
    win_p = np.zeros((D, 4096), f32)
    win_p[:, :perm.size] = w_in[:, perm]
    win_p[:, 2048:3072] = w_in[:, ga]
    win_p[:, 3072:4096] = w_in[:, gb]

    ws = np.zeros((NSLOT, 128, 4096), f32)

    def gu_slots(wg, wu, base):
        wg = np.asarray(wg, f32).reshape(8, 128, NJ, 128)
        wu = np.asarray(wu, f32).reshape(8, 128, NJ, 128)
        both = np.stack([wg, wu], 0)
        both = both.reshape(2, 8, 128, 11, 2, 128)
        arr = both.transpose(3, 2, 4, 0, 1, 5)
        ws[base:base + 11] = arr.reshape(11, 128, 4096)

    gu_slots(inp["w_ffn1_gate"][0], inp["w_ffn1_up"][0], 0)
    gu_slots(inp["w_ffn2_gate"][0], inp["w_ffn2_up"][0], 23)
    wp = win_p.reshape(8, 128, 8, 512)
    ws[11:19] = wp.transpose(2, 1, 0, 3).reshape(8, 128, 4096)
    heads = [2 * (sl % 4) + sl // 4 for sl in range(8)]
    rperm = np.concatenate([np.arange(h * 64, (h + 1) * 64) for h in heads])
    for k, name in enumerate(("w_branch_sparse", "w_branch_swa")):
        wb = np.asarray(inp[name][0], f32)[rperm]
        ws[19 + k] = wb.reshape(4, 128, 1024).transpose(1, 0, 2).reshape(128, 4096)
    wo = np.asarray(inp["w_out"][0], f32).reshape(8, 128, 1024)
    ws[21] = wo[0:4].transpose(1, 0, 2).reshape(128, 4096)
    ws[22] = wo[4:8].transpose(1, 0, 2).reshape(128, 4096)

    wd = np.stack([np.asarray(inp["w_ffn1_down"][0], f32).reshape(NJ, 128, 1024).transpose(1, 0, 2).reshape(128, NJ * 1024),
                   np.asarray(inp["w_ffn2_down"][0], f32).reshape(NJ, 128, 1024).transpose(1, 0, 2).reshape(128, NJ * 1024)], 0)

    gcol = np.concatenate([np.asarray(inp[k][0], f32).reshape(8, 128).T for k in ("norm_ffn1", "norm_mix", "norm_ffn2")], 1)
    sink = np.asarray(inp["sinks"][0], f32)[heads]

    pos = np.maximum(np.arange(NBLK * 128, dtype=np.int32) - 112, 0).astype(f32)
    inv_freq = (1.0 / (np.float32(10000.0) ** (np.arange(0, 64, 2, dtype=f32) / np.float32(64)))).astype(f32)
    ang = (pos[:, None] * inv_freq[None, :]).astype(f32)
    cs = np.concatenate([np.cos(ang).astype(f32), np.sin(ang).astype(f32)], 1)

    q = np.arange(128)[:, None]
    s = np.arange(128)[None, :]
    cst = np.zeros((128, 1152), f32)
    cst[:, 0:512] = np.tile(np.eye(128, dtype=f32), (1, 4))
    cst[:, 512:640] = np.where(s <= q, 0.0, -1e30)
    cst[:, 640:768] = np.where(s <= q, 0.0, NEGM)
    cst[:, 768:896] = np.where(s > q, 0.0, NEGM)
    cst[:, 896:1024] = np.where((s > q) & (s >= 112), 0.0, NEGM)
    p2 = np.tile((2.0 ** -(np.arange(32, dtype=np.float64) + 1)).astype(f32)[None, :], (128, 1))
    shared = {"meta": np.ascontiguousarray(np.asarray(inp["meta_tokens"], f32)), "ws": ws, "wd": np.ascontiguousarray(wd),
              "gcol": np.ascontiguousarray(gcol), "gfin": np.asarray(inp["norm_final"], f32), "sink": np.ascontiguousarray(sink),
              "cs": cs, "cst": cst, "p2": p2}
    return shared


_NC_CACHE = {}


def kernel(**inputs):
    shared = _host_layout(inputs)
    x = np.asarray(inputs["x"], np.float32)
    if "nc" not in _NC_CACHE:
        _NC_CACHE["nc"] = build_program()
    nc = _NC_CACHE["nc"]
    in_maps = []
    for b in range(8):
        m = dict(shared)
        m["x"] = np.ascontiguousarray(x[b])
        in_maps.append(m)
    res = run_bass_kernel_spmd(nc, in_maps, core_ids=list(range(8)))
    return np.stack([np.asarray(r["out"], np.float32) for r in res.results], 0)
```

```python
import numpy as np
from contextlib import ExitStack
import concourse.bass as bass
import concourse.mybir as mybir
from concourse.bass_utils import run_bass_kernel_spmd

F32 = mybir.dt.float32
BF16 = mybir.dt.bfloat16
AF = mybir.ActivationFunctionType
ALU = mybir.AluOpType
AX = mybir.AxisListType

D = 1024
SEQ = 4096
NBLK = 33
FF = 2816
NJ = 22
NSLOT = 34
NIT = 16
EPS = 1e-6
IDX_SCALE = (4 ** -0.5) * (64 ** -0.5)
NEGM = -30000.0


class TT:
    __slots__ = ("name", "w", "r")

    def __init__(self, name):
        self.name = name
        self.w = None
        self.r = {}


class Sched:
    ENGS = ("pe", "act", "dve", "pool", "sp")

    def __init__(self, nc, n_dma_sems=12):
        self.nc = nc
        self.ops = {e: [] for e in self.ENGS}
        self.cnt = {e: 0 for e in self.ENGS}
        self.seen = {e: {} for e in self.ENGS}
        self.n_dma = n_dma_sems
        self.dma_val = {}
        self.dma_rr = {"sp": 0, "pool": 0, "act": 0}
        self.sems = {}
        self.final_waits = []

    def _need(self, eng, deps, key, val):
        if self.seen[eng].get(key, 0) >= val:
            return
        deps[key] = max(deps.get(key, 0), val)

    def _deps(self, eng, reads, writes, is_dma=False):
        deps = {}
        for t in reads:
            if t.w is not None:
                self._need(eng, deps, t.w[0], t.w[1])
        for t in writes:
            if t.w is not None and (is_dma or t.w[0] != eng):
                self._need(eng, deps, t.w[0], t.w[1])
            for k, v in t.r.items():
                if is_dma or k != eng:
                    self._need(eng, deps, k, v)
        for k, v in deps.items():
            self.seen[eng][k] = v
        return list(deps.items())

    def op(self, eng, fn, reads=(), writes=()):
        waits = self._deps(eng, reads, writes)
        self.cnt[eng] += 1
        c = self.cnt[eng]
        self.ops[eng].append((waits, fn, (eng, 1)))
        for t in reads:
            t.r[eng] = c
        for t in writes:
            t.w = (eng, c)
            t.r = {}
        return c

    def dma(self, q, fn, reads=(), writes=()):
        s = self.dma_rr[q]
        self.dma_rr[q] = (s + 1) % self.n_dma
        key = "d%s%d" % (q, s)
        s = key
        waits = self._deps(q, reads, writes, is_dma=True)
        prev = self.dma_val.get(s, 0)
        if prev > 0 and self.seen[q].get(key, 0) < prev:
            waits.append((key, prev))
            self.seen[q][key] = prev
        self.dma_val[s] = prev + 16
        v = self.dma_val[s]
        self.ops[q].append((waits, fn, (key, 16)))
        for t in reads:
            t.r[key] = v
        for t in writes:
            t.w = (key, v)
            t.r = {}

    def finish(self, eng, tiles):
        waits = [t.w for t in tiles if t.w is not None]
        self.final_waits.append((eng, waits))

    def emit(self, stack):
        nc = self.nc
        keys = list(self.ENGS) + sorted(self.dma_val.keys())
        for k in keys:
            self.sems[k] = stack.enter_context(nc.semaphore("s_" + k))
        block = stack.enter_context(nc.Block())
        sems = self.sems

        def run(eng_name):
            def body(e):
                for waits, fn, inc in self.ops[eng_name]:
                    for k, v in waits:
                        e.wait_ge(sems[k], v)
                    ins = fn(e)
                    ins.then_inc(sems[inc[0]], inc[1])
                for en, waits in self.final_waits:
                    if en == eng_name:
                        for k, v in waits:
                            e.wait_ge(sems[k], v)
            return body

        block.tensor(run("pe"))
        block.scalar(run("act"))
        block.vector(run("dve"))
        block.gpsimd(run("pool"))
        block.sync(run("sp"))


def build_program(stage=99):
    nc = bass.Bass("TRN2", target_bir_lowering=False, dynamic_dma_scratch_size=2048)
    dt = nc.dram_tensor
    x_d = dt("x", [SEQ, D], F32, kind="ExternalInput").ap()
    meta_d = dt("meta", [16, D], F32, kind="ExternalInput").ap()
    ws_d = dt("ws", [NSLOT, 128, 4096], F32, kind="ExternalInput").ap()
    wd_d = dt("wd", [2, 128, NJ * 1024], F32, kind="ExternalInput").ap()
    gcol_d = dt("gcol", [128, 24], F32, kind="ExternalInput").ap()
    gfin_d = dt("gfin", [D], F32, kind="ExternalInput").ap()
    sink_d = dt("sink", [8], F32, kind="ExternalInput").ap()
    cs_d = dt("cs", [NBLK * 128, 64], F32, kind="ExternalInput").ap()
    cst_d = dt("cst", [128, 1152], F32, kind="ExternalInput").ap()
    p2_d = dt("p2", [128, 32], F32, kind="ExternalInput").ap()
    wsb_d = dt("wsb", [NSLOT, 128, 4096], BF16, kind="Internal").ap()
    wdb_d = dt("wdb", [2, 128, NJ * 1024], BF16, kind="Internal").ap()
    out_d = dt("out", [SEQ, D], F32, kind="ExternalOutput").ap()

    with ExitStack() as st:
        def sb(n, s, d):
            return st.enter_context(nc.sbuf_tensor(n, s, d))

        def ps(n, s, d):
            return st.enter_context(nc.psum_tensor(n, s, d))

        PA = ps("PA", [128, 4, 512], F32)
        PB = ps("PB", [128, 2, 512], F32)
        PTR = ps("PTR", [128, 2, 1024], BF16)

        X = sb("X", [128, 4, 1024], F32)
        XT = sb("XT", [128, 8, 512], BF16)
        BIG = sb("BIG", [128, 7168], F32)
        AT = BIG[:].bitcast(BF16)
        SG = sb("SG", [128, 2, 512], BF16)
        WR = sb("WR", [128, 3, 4096], BF16)
        WD = sb("WD", [128, NJ * 1024], BF16)
        SGA = sb("SGA", [128, 4, 1024], BF16)
        SGB = sb("SGB", [128, 4, 1024], BF16)
        ROT = sb("ROT", [128, 28, 64], BF16)
        QT = sb("QT", [128, 14, 128], BF16)
        KST = sb("KST", [128, 2, 2, 128], BF16)
        KAT = sb("KAT", [128, NBLK * 128], BF16)
        KIT = sb("KIT", [128, NBLK * 128], BF16)
        VA = sb("VA", [128, NBLK, 66], BF16)
        VS = sb("VS", [128, 5, 2, 66], BF16)
        SC = sb("SC", [128, NBLK * 128], F32)
        NEG = sb("NEG", [128, NBLK * 128], BF16)
        RL = sb("RL", [128, 4, 512], F32)
        PT = sb("PT", [128, 2, 1024], BF16)
        OSP = sb("OSP", [128, 512], BF16)
        OWP = sb("OWP", [128, 512], BF16)
        OT = sb("OT", [128, 8, 128], BF16)
        MG = sb("MG", [128, 1024], BF16)
        CAUSF = sb("CAUSF", [128, 128], F32)
        CB = sb("CB", [128, 1152], BF16)
        GFIN = sb("GFIN", [128, 1024], F32)
        CS = sb("CS", [128, 4, 64], F32)
        G = sb("G", [128, 24], F32)
        P2 = sb("P2", [128, 32], F32)
        SM = sb("SM", [128, 64], F32)
        WT = sb("WT", [128, 32], F32)
        WI = sb("WI", [128, 4, 4], F32)
        REC = sb("REC", [128, 8], F32)
        ESINK = sb("ESINK", [128, 8], F32)

        SS = SM[:, 0:4]
        MS = SM[:, 4:8]
        RSTD = SM[:, 8:12]
        MHALF = SM[:, 12:16]
        AMAX = SM[:, 16:17]
        LO = SM[:, 17:18]
        RNG = SM[:, 18:19]
        MID = SM[:, 19:20]
        CNT = SM[:, 20:21]
        TV = SM[:, 21:22]
        THR = SM[:, 22:23]
        DEN = SM[:, 24:32]
        SA = SM[:, 32:33]

        NEG1 = WD[:, 0:4224]
        QT1 = WD[:, 4224:6016].rearrange("p (g t) -> p g t", g=14)
        KSTW = WD[:, 6016:7040].rearrange("p (a k t) -> p a k t", a=4, k=2)
        QT2 = WD[:, 7040:8832].rearrange("p (g t) -> p g t", g=14)
        QTs = [QT, QT1, QT2]
        OSPW = WD[:, 8832:10880].rearrange("p (i c) -> p i c", i=4)
        OWPW = WD[:, 10880:12928].rearrange("p (i c) -> p i c", i=4)
        OT2 = WD[:, 12928:13952].rearrange("p (k t) -> p k t", k=8)
        MG2 = WD[:, 13952:14976]
        OTs = [OT[:], OT2]
        MGs = [MG[:], MG2]
        NEGs = [NEG, NEG1]
        S = Sched(nc)
        T = TT
        tX = [T("X%d" % i) for i in range(4)]
        tXT = T("XT")
        tATc = [T("AT%d" % j) for j in range(NJ)]
        tR = [T("R%d" % i) for i in range(4)]
        tSG = [T("SG0"), T("SG1")]
        tWR = [T("WR%d" % i) for i in range(3)]
        tWD = T("WD")
        tSGA = [T("SGA%d" % i) for i in range(4)]
        tSGB = [T("SGB%d" % i) for i in range(4)]
        tROT = T("ROT")
        tQTs = [T("QT0"), T("QT1"), T("QT2")]
        tNEGs = [T("NEG0"), T("NEG1")]
        tNEGAs = [T("NEGA0"), T("NEGA1")]
        tKSTW = [T("KSTW%d" % i) for i in range(4)]
        tOSPs = [T("OSP%d" % i) for i in range(4)]
        tOWPs = [T("OWP%d" % i) for i in range(4)]
        tOTs = [T("OT0"), T("OT1")]
        tMGs = [T("MG0"), T("MG1")]
        tKST0 = T("KST0")
        tKAT, tKIT, tVA = T("KAT"), T("KIT"), T("VA")
        tVS = [T("VS%d" % i) for i in range(5)]
        tSC = T("SC")
        tSA, tMID = T("SA"), T("MID")
        tRL = [T("RL%d" % i) for i in range(4)]
        tPT = [T("PT0"), T("PT1")]
        tOSP, tOWP, tOT, tMG = T("OSP"), T("OWP"), T("OT"), T("MG")
        tC = T("CONST")
        tCS = T("CS")
        tSM = T("SM")
        tBI = T("BI")
        tWI = T("WI")
        tREC = T("REC")
        tPA = [T("PA%d" % i) for i in range(4)]
        tPB = [T("PB0"), T("PB1")]
        tPTR = [T("PTR0"), T("PTR1")]
        tWSB = [T("wsb%d" % i) for i in range(NSLOT)]
        tWDB = [T("wdb0"), T("wdb1")]
        tOUT = [T("out%d" % i) for i in range(40)]
        tF = [T("F%d" % i) for i in range(4)]

        CF = RL[:].rearrange("p a b -> p (a b)")[:, 0:1152]
        S.dma("sp", lambda e: e.dma_start(out=CF, in_=cst_d), writes=[tC] + tRL)
        S.dma("sp", lambda e: e.dma_start(out=CAUSF[:], in_=cst_d[:, 512:640]), writes=[tC])
        S.dma("sp", lambda e: e.dma_start(out=G[:], in_=gcol_d), writes=[tC])
        S.dma("sp", lambda e: e.dma_start(out=P2[:], in_=p2_d), writes=[tC])
        S.dma("sp", lambda e: e.dma_start(out=GFIN[:], in_=gfin_d.partition_broadcast(128)), writes=[tC])
        S.dma("sp", lambda e: e.dma_start(out=ESINK[:], in_=sink_d.partition_broadcast(128)), writes=[tC])
        S.op("dve", lambda e: e.tensor_copy(out=CB[:], in_=CF), reads=[tC] + tRL, writes=[tC])
        S.op("act", lambda e: e.activation(out=ESINK[:], in_=ESINK[:], func=AF.Exp), reads=[tC], writes=[tC])
        S.op("pool", lambda e: e.memset(SM[:, 12:16], -0.5), writes=[tSM])
        S.op("pool", lambda e: e.memset(VA[:, :, 64:66], 1.0), writes=[tVA])
        S.op("pool", lambda e: e.memset(VS[:, :, :, 64:66], 1.0), writes=tVS)
        ID4 = CB[:, 0:512]
        CAUS = CAUSF[:]
        SWCUR = CB[:, 640:768]
        SWPREV = CB[:, 768:896]
        SWPREV1 = CB[:, 896:1024]

        def convert_group(conv_order, first_reads=()):
          fr = list(first_reads)
          for it in conv_order:
            if isinstance(it, str):
                f = int(it[2])
                for hlf in range(2):
                    sl = slice(hlf * 11 * 1024, (hlf + 1) * 11 * 1024)
                    S.dma("pool", lambda e, f=f, sl=sl: e.dma_start(out=wdb_d[f, :, sl], in_=wd_d[f, :, sl], max_dma_last_dim=8192), writes=[tWDB[f]])
            else:
                S.dma("pool", lambda e, it=it: e.dma_start(out=wsb_d[it], in_=ws_d[it], max_dma_last_dim=8192), reads=fr, writes=[tWSB[it]])
                fr = []

        tiles = [[0]] + [list(range(1 + 4 * t, 5 + 4 * t)) for t in range(8)]
        seq = []
        for ti, blks in enumerate(tiles):
            if ti == 0:
                seq += list(range(0, 15))
            else:
                seq += list(range(0, NSLOT))
        ring = {"issued": 0, "used": 0, "holds": set()}

        def issue_next():
            k = ring["issued"]
            if k >= len(seq):
                return
            pos = k % 3
            slot = seq[k]
            S.dma("sp", lambda e, pos=pos, slot=slot: e.dma_start(out=WR[:, pos, :], in_=wsb_d[slot]),
                  reads=[tWSB[slot]], writes=[tWR[pos]])
            ring["issued"] += 1

        def pump():
            k = ring["used"] - 1
            released = min([k] + list(ring["holds"]))
            while ring["issued"] < min(k + 3, released + 3, len(seq)):
                issue_next()

        def use_slot(expect, hold=False):
            k = ring["used"]
            assert seq[k] == expect, (k, seq[k], expect)
            ring["used"] += 1
            if hold:
                ring["holds"].add(k)
            pump()
            assert ring["issued"] > k
            return k % 3

        def unhold_all():
            ring["holds"].clear()
            pump()

        def load_wd(f, alias=False):
            wr = [tWD] + ((tQTs + tNEGs + tNEGAs + tKSTW + tOSPs + tOWPs + [tOTs[1], tMGs[1]]) if alias else [])
            S.dma("pool", lambda e, f=f: e.dma_start(out=WD[:], in_=wdb_d[f]), reads=[tWDB[f]], writes=wr)

        SGf = SG[:].rearrange("p a b -> p (a b)")
        XSs = [PT[:, 0, :], PT[:, 1, :]]

        def norm_to_xt(NB, gi, ptr_par):
            for i in range(NB):
                S.op("act", lambda e, i=i: e.activation(out=SGf, in_=X[:, i, :], func=AF.Square, accum_out=SS[:, i:i + 1]),
                     reads=[tX[i]], writes=[tSG[0], tSG[1], tSM])
            S.op("dve", lambda e: e.tensor_scalar(out=MS[:, 0:NB], in0=SS[:, 0:NB], scalar1=1.0 / D, scalar2=EPS, op0=ALU.mult, op1=ALU.add),
                 reads=[tSM], writes=[tSM])
            S.op("pool", lambda e: e.tensor_tensor(out=RSTD[:, 0:NB], in0=MS[:, 0:NB], in1=MHALF[:, 0:NB], op=ALU.pow),
                 reads=[tSM], writes=[tSM])
            for i in range(NB):
                xs, txs = XSs[i % 2], tPT[i % 2]
                S.op("act", lambda e, i=i, xs=xs: e.mul(out=xs, in_=X[:, i, :], mul=RSTD[:, i:i + 1]), reads=[tX[i], tSM], writes=[txs])
                par = (ptr_par + i) % 2

                def tr(e, par=par, xs=xs):
                    ins = None
                    for kc in range(8):
                        ins = e.transpose(out=PTR[:, par, kc * 128:(kc + 1) * 128], in_=xs[:, kc * 128:(kc + 1) * 128], identity=CB[:, 0:128])
                    return ins
                S.op("pe", tr, reads=[txs, tC], writes=[tPTR[par]])
                S.op("dve", lambda e, i=i, par=par: e.tensor_tensor(
                    out=XT[:, :, i * 128:(i + 1) * 128],
                    in0=PTR[:, par, :].rearrange("p (k t) -> p k t", k=8),
                    in1=G[:, gi * 8:(gi + 1) * 8].unsqueeze(2).to_broadcast([128, 8, 128]), op=ALU.mult),
                    reads=[tPTR[par], tC], writes=[tXT])

        def ffn(f, NB, alias_tiles):
            TK = NB * 128
            base = 0 if f == 0 else 23
            first = True
            for j in range(NJ):
                if j % 2 == 0:
                    pos = use_slot(base + j // 2)
                cj = j % 2
                gp = j % 2
                woff = cj * 2048

                def gu(e, pos=pos, woff=woff, gp=gp):
                    ins = None
                    for g in range(2):
                        for kc in range(8):
                            o = woff + g * 1024 + kc * 128
                            ins = e.matmul(PA[:, g * 2 + gp, 0:TK], lhsT=WR[:, pos, o:o + 128], rhs=XT[:, kc, 0:TK], start=(kc == 0), stop=(kc == 7))
                    return ins
                S.op("pe", gu, reads=[tWR[pos], tXT], writes=[tPA[gp], tPA[2 + gp]])
                S.op("act", lambda e, gp=gp: e.activation(out=SG[:, gp, 0:TK], in_=PA[:, gp, 0:TK], func=AF.Silu), reads=[tPA[gp]], writes=[tSG[gp]])
                wr = [tATc[j]] + (alias_tiles if first else [])
                first = False
                S.op("dve", lambda e, j=j, gp=gp: e.tensor_tensor(out=AT[:, j * 512:j * 512 + TK], in0=PA[:, 2 + gp, 0:TK], in1=SG[:, gp, 0:TK], op=ALU.mult),
                     reads=[tPA[2 + gp], tSG[gp]], writes=wr)
            n = 0
            for i in range(NB):
                for half in range(2):
                    par = n % 2
                    n += 1

                    def dn(e, i=i, half=half, par=par):
                        ins = None
                        for j in range(NJ):
                            ins = e.matmul(PB[:, par, :], lhsT=AT[:, j * 512 + i * 128:j * 512 + (i + 1) * 128],
                                           rhs=WD[:, j * 1024 + half * 512:j * 1024 + (half + 1) * 512], start=(j == 0), stop=(j == NJ - 1))
                        return ins
                    S.op("pe", dn, reads=tATc + [tWD], writes=[tPB[par]])
                    S.op("dve", lambda e, i=i, half=half, par=par: e.scalar_tensor_tensor(
                        out=X[:, i, half * 512:(half + 1) * 512], in0=PB[:, par, :], scalar=0.5, in1=X[:, i, half * 512:(half + 1) * 512],
                        op0=ALU.mult, op1=ALU.add), reads=[tPB[par], tX[i]], writes=[tX[i]])

        Rv = BIG[:].rearrange("p (i c) -> p i c", i=4)

        def project(NB, blks, chunks):
            n = 0
            for cc in chunks:
                pos = use_slot(11 + cc)
                for i in range(NB):
                    par = n % 2
                    n += 1
                    c = blks[i]

                    def pj(e, pos=pos, i=i, par=par):
                        ins = None
                        for kc in range(8):
                            ins = e.matmul(PB[:, par, :], lhsT=XT[:, kc, i * 128:(i + 1) * 128], rhs=WR[:, pos, kc * 512:(kc + 1) * 512], start=(kc == 0), stop=(kc == 7))
                        return ins
                    S.op("pe", pj, reads=[tWR[pos], tXT], writes=[tPB[par]])
                    if cc < 3:
                        wr = [tR[i]] + (tATc if cc == 0 and i == 0 else [])
                        eng = "act" if (n % 2 == 0) else "dve"
                        if eng == "act":
                            S.op("act", lambda e, i=i, cc=cc, par=par: e.copy(out=Rv[:, i, cc * 512:(cc + 1) * 512], in_=PB[:, par, :]), reads=[tPB[par]], writes=wr)
                        else:
                            S.op("dve", lambda e, i=i, cc=cc, par=par: e.tensor_copy(out=Rv[:, i, cc * 512:(cc + 1) * 512], in_=PB[:, par, :]), reads=[tPB[par]], writes=wr)
                    elif cc == 3:
                        S.op("act", lambda e, i=i, par=par: e.copy(out=Rv[:, i, 1536:1792], in_=PB[:, par, 0:256]), reads=[tPB[par]], writes=[tR[i]])
                        S.op("act", lambda e, c=c, par=par: e.copy(out=VA[:, c, 0:64], in_=PB[:, par, 256:320]), reads=[tPB[par]], writes=[tVA])
                        S.op("act", lambda e, i=i, par=par: e.copy(out=VS[:, i + 1, :, 0:64], in_=PB[:, par, 320:448].rearrange("p (k d) -> p k d", k=2)),
                             reads=[tPB[par]], writes=[tVS[i + 1]])
                        S.op("act", lambda e, i=i, par=par: e.mul(out=WI[:, i, :], in_=PB[:, par, 448:452], mul=IDX_SCALE),
                             reads=[tPB[par]], writes=[tWI])
                    else:
                        gsel = (cc - 4) // 2
                        hh = (cc - 4) % 2
                        dst = SGA if gsel == 0 else SGB
                        tdst = tSGA if gsel == 0 else tSGB
                        S.op("act", lambda e, dst=dst, i=i, hh=hh, par=par: e.activation(out=dst[:, i, hh * 512:(hh + 1) * 512], in_=PB[:, par, :], func=AF.Sigmoid),
                             reads=[tPB[par]], writes=[tdst[i]])
                    yield

        T1 = RL[:, 0:2, :].rearrange("p a b -> p (a b)")[:, 0:896].rearrange("p (h d) -> p h d", h=28)
        T2 = RL[:, 2:4, :].rearrange("p a b -> p (a b)")[:, 0:896].rearrange("p (h d) -> p h d", h=28)

        def rope_block(i, c, tile0=False):
            QTx, tQ = QTs[i % 3], tQTs[i % 3]
            Rb = Rv[:, i, :].rearrange("p (h t d) -> p h t d", h=28, t=2)
            x1 = Rb[:, :, 0, :]
            x2 = Rb[:, :, 1, :]
            cos = CS[:, i, 0:32].unsqueeze(1).to_broadcast([128, 28, 32])
            sin = CS[:, i, 32:64].unsqueeze(1).to_broadcast([128, 28, 32])
            rl01, rl23 = [tRL[0], tRL[1]], [tRL[2], tRL[3]]
            P = "pool"
            S.op(P, lambda e: e.tensor_tensor(out=T1, in0=x1, in1=cos, op=ALU.mult), reads=[tR[i], tCS], writes=rl01)
            S.op(P, lambda e: e.tensor_tensor(out=T2, in0=x2, in1=sin, op=ALU.mult), reads=[tR[i], tCS], writes=rl23)
            S.op(P, lambda e: e.tensor_tensor(out=ROT[:, :, 0:32], in0=T1, in1=T2, op=ALU.subtract), reads=rl01 + rl23, writes=[tROT])
            S.op(P, lambda e: e.tensor_tensor(out=T1, in0=x2, in1=cos, op=ALU.mult), reads=[tR[i], tCS], writes=rl01)
            S.op(P, lambda e: e.tensor_tensor(out=T2, in0=x1, in1=sin, op=ALU.mult), reads=[tR[i], tCS], writes=rl23)
            S.op(P, lambda e: e.tensor_tensor(out=ROT[:, :, 32:64], in0=T1, in1=T2, op=ALU.add), reads=rl01 + rl23, writes=[tROT])
            ROTf = ROT[:].rearrange("p h d -> p (h d)")

            def tr(e):
                ins = None
                for g in range(14):
                    ins = e.transpose(out=PTR[:, g // 8, (g % 8) * 128:(g % 8 + 1) * 128], in_=ROTf[:, g * 128:(g + 1) * 128], identity=CB[:, 0:128])
                return ins
            S.op("pe", tr, reads=[tROT, tC], writes=[tPTR[0], tPTR[1]])
            S.op("act", lambda e: e.copy(out=QTx[:, 0:8, :], in_=PTR[:, 0, :].rearrange("p (g t) -> p g t", g=8)), reads=[tPTR[0]], writes=[tQ])
            S.op("dve", lambda e: e.tensor_copy(out=QTx[:, 8:14, :], in_=PTR[:, 1, 0:768].rearrange("p (g t) -> p g t", g=6)), reads=[tPTR[1]], writes=[tQ])
            S.op("pool", lambda e: e.tensor_copy(out=KAT[:, c * 128:(c + 1) * 128], in_=QTx[:, 6, :]), reads=[tQ], writes=[tKAT])
            S.op("pool", lambda e: e.tensor_copy(out=KIT[:, c * 128:(c + 1) * 128], in_=QTx[:, 7, :]), reads=[tQ], writes=[tKIT])
            if tile0 or i == 3:
                S.op("pool", lambda e: e.tensor_copy(out=KST[:, 0, :, :], in_=QTx[:, 12:14, :]), reads=[tQ], writes=[tKST0])
            if not tile0:
                S.op("pool", lambda e: e.tensor_copy(out=KSTW[:, i, :, :], in_=QTx[:, 12:14, :]), reads=[tQ], writes=[tKSTW[i]])

        def attn_core(keyblocks, out_sb, t_out, sink, QTx, tQ):
            nk = len(keyblocks)

            def emit_sc(jj):
                s = jj % 2
                kfn, negap, vfn, rds, swa = keyblocks[jj]

                def sc(e):
                    ins = None
                    for hf in range(2):
                        if not swa:
                            e.matmul(PA[:, 2 * s + hf, :], lhsT=kfn(hf, 0), rhs=QTx[hf * 64:(hf + 1) * 64, 0:4, :], start=True, stop=False)
                        else:
                            for kap in range(2):
                                e.matmul(PA[:, 2 * s + hf, kap * 256:(kap + 1) * 256], lhsT=kfn(hf, kap),
                                         rhs=QTx[hf * 64:(hf + 1) * 64, 8 + 2 * kap:10 + 2 * kap, :], start=(kap == 0), stop=False, skip_group_check=True)
                    for hf in range(2):
                        ins = e.matmul(PA[:, 2 * s + hf, :], lhsT=negap, rhs=ID4, start=False, stop=True, skip_group_check=True)
                    return ins
                S.op("pe", sc, reads=rds + [tQ, tC], writes=[tPA[2 * s], tPA[2 * s + 1]])

            def emit_exp_pv(jj):
                s = jj % 2
                kfn, negap, vfn, rds, swa = keyblocks[jj]
                S.op("act", lambda e: e.activation(out=PT[:, s, :], in_=PA[:, 2 * s:2 * s + 2, :].rearrange("p a b -> p (a b)"), func=AF.Exp, scale=0.125),
                     reads=[tPA[2 * s], tPA[2 * s + 1]], writes=[tPT[s]])

                def pv(e):
                    ins = None
                    for hf in range(2):
                        for pr in range(4):
                            ins = e.matmul(PB[:, hf, pr * 65:(pr + 1) * 65], lhsT=PT[:, s, (hf * 4 + pr) * 128:(hf * 4 + pr + 1) * 128], rhs=vfn(pr),
                                           start=(jj == 0 and pr == 0), stop=(jj == nk - 1 and pr == 3), skip_group_check=True)
                    return ins
                S.op("pe", pv, reads=rds + [tPT[s]], writes=[tPB[0], tPB[1]])

            emit_sc(0)
            for jj in range(nk):
                if jj + 1 < nk:
                    emit_sc(jj + 1)
                emit_exp_pv(jj)
                yield
            PBv = PB[:, :, 0:260].rearrange("p b (r e) -> p b r e", e=65)
            RECv = REC[:].rearrange("p (b r o) -> p b r o", b=2, o=1)
            if sink:
                S.op("dve", lambda e: e.tensor_tensor(out=RECv, in0=PBv[:, :, :, 64:65], in1=ESINK[:].rearrange("p (b r o) -> p b r o", b=2, o=1), op=ALU.add),
                     reads=[tPB[0], tPB[1], tC], writes=[tREC])
                S.op("dve", lambda e: e.reciprocal(out=REC[:], in_=REC[:]), reads=[tREC], writes=[tREC])
            else:
                S.op("dve", lambda e: e.reciprocal(out=RECv, in_=PBv[:, :, :, 64:65]), reads=[tPB[0], tPB[1]], writes=[tREC])
            S.op("dve", lambda e: e.tensor_tensor(out=out_sb[:].rearrange("p (b r d) -> p b r d", b=2, r=4), in0=PBv[:, :, :, 0:64],
                                                  in1=RECv.to_broadcast([128, 2, 4, 64]), op=ALU.mult),
                 reads=[tPB[0], tPB[1], tREC], writes=[t_out])

        def index_bisect(i, c, frac=0.42):
            split = frac < 1.0
            QTx, tQ = QTs[i % 3], tQTs[i % 3]
            NEGx, tNEG, tNEGA = NEGs[i % 2], tNEGs[i % 2], tNEGAs[i % 2]
            n = (c + 1) * 128
            nch = (n + 511) // 512
            for ch in range(nch):
                w = min(512, n - ch * 512)

                for pr in range(2):
                    def ix(e, ch=ch, w=w, pr=pr):
                        ins = None
                        for hf in range(2):
                            ins = e.matmul(PA[:, 2 * pr + hf, 0:w], lhsT=QTx[hf * 64:(hf + 1) * 64, 4 + pr, :],
                                           rhs=KIT[hf * 64:(hf + 1) * 64, ch * 512:ch * 512 + w], start=True, stop=True)
                        return ins
                    S.op("pe", ix, reads=[tQ, tKIT], writes=[tPA[2 * pr], tPA[2 * pr + 1]])
                for h in range(4):
                    S.op("act", lambda e, h=h, w=w: e.activation(out=RL[:, h, 0:w], in_=PA[:, h, 0:w], func=AF.Relu), reads=[tPA[h]], writes=[tRL[h]])
                S.op("dve", lambda e, ch=ch, w=w: e.tensor_scalar(out=SC[:, ch * 512:ch * 512 + w], in0=RL[:, 0, 0:w], scalar1=WI[:, i, 0:1], scalar2=None, op0=ALU.mult),
                     reads=[tRL[0], tWI], writes=[tSC])
                for h in range(1, 4):
                    S.op("dve", lambda e, ch=ch, w=w, h=h: e.scalar_tensor_tensor(out=SC[:, ch * 512:ch * 512 + w], in0=RL[:, h, 0:w], scalar=WI[:, i, h:h + 1],
                                                                                 in1=SC[:, ch * 512:ch * 512 + w], op0=ALU.mult, op1=ALU.add),
                         reads=[tRL[h], tWI, tSC], writes=[tSC])
            V = "dve"
            S.op(V, lambda e: e.tensor_reduce(out=AMAX, in_=SC[:, 0:n], axis=AX.X, op=ALU.max, apply_absolute_value=True), reads=[tSC], writes=[tBI])
            S.op(V, lambda e: e.memset(SC[:, 0:112], -1e30), reads=[tSC], writes=[tSC])
            S.op(V, lambda e: e.tensor_tensor(out=SC[:, c * 128:(c + 1) * 128], in0=SC[:, c * 128:(c + 1) * 128], in1=CAUS, op=ALU.add), reads=[tSC, tC], writes=[tSC])
            S.op(V, lambda e: e.tensor_scalar(out=LO, in0=AMAX, scalar1=-1.001, scalar2=-1e-20, op0=ALU.mult, op1=ALU.add), reads=[tBI], writes=[tBI])
            S.op(V, lambda e: e.tensor_tensor(out=RNG, in0=AMAX, in1=LO, op=ALU.subtract), reads=[tBI], writes=[tBI])
            S.op(V, lambda e: e.tensor_scalar(out=WT[:, 0:NIT + 1], in0=P2[:, 0:NIT + 1], scalar1=RNG, scalar2=None, op0=ALU.mult), reads=[tBI, tC], writes=[tBI])
            S.op(V, lambda e: e.tensor_tensor(out=MID, in0=LO, in1=WT[:, 0:1], op=ALU.add), reads=[tBI], writes=[tMID])
            nd = (min(c, max(1, int(frac * (c + 1) + 0.5))) * 128) if split else n
            na = n - nd
            for it in range(NIT):
                if split:
                    S.op("act", lambda e: e.activation(out=NEGx[:, nd:n], in_=SC[:, nd:n], func=AF.Sign, bias=MID, scale=-1.0, accum_out=SA),
                         reads=[tSC, tMID], writes=[tNEGA, tSA])
                S.op(V, lambda e: e.tensor_scalar(out=NEGx[:, 0:nd], in0=SC[:, 0:nd], scalar1=MID, scalar2=None, op0=ALU.is_gt, op1=ALU.add, accum_out=CNT),
                     reads=[tSC, tMID], writes=[tNEG, tBI])
                if split:
                    S.op(V, lambda e: e.scalar_tensor_tensor(out=CNT, in0=SA, scalar=-0.5, in1=CNT, op0=ALU.mult, op1=ALU.add), reads=[tSA, tBI], writes=[tBI])
                S.op(V, lambda e: e.tensor_scalar(out=TV, in0=CNT, scalar1=255.5 - na / 2.0, scalar2=0.5, op0=ALU.is_gt, op1=ALU.subtract), reads=[tBI], writes=[tBI])
                S.op(V, lambda e, it=it: e.scalar_tensor_tensor(out=MID, in0=TV, scalar=WT[:, it:it + 1], in1=MID, op0=ALU.mult, op1=ALU.add), reads=[tBI, tMID], writes=[tMID])
                yield
            S.op(V, lambda e: e.tensor_tensor(out=THR, in0=MID, in1=WT[:, NIT:NIT + 1], op=ALU.subtract), reads=[tBI, tMID], writes=[tBI])
            S.op(V, lambda e: e.tensor_scalar(out=NEGx[:, 0:n], in0=SC[:, 0:n], scalar1=THR, scalar2=NEGM, op0=ALU.is_le, op1=ALU.mult), reads=[tSC, tBI], writes=[tNEG, tNEGA])

        def attn_main(i, c):
            QTx, tQ = QTs[i % 3], tQTs[i % 3]
            NEGx, tNEG, tNEGA = NEGs[i % 2], tNEGs[i % 2], tNEGAs[i % 2]
            kbs = []
            for j in range(c + 1):
                kbs.append((lambda hf, kap, j=j: KAT[hf * 64:(hf + 1) * 64, j * 128:(j + 1) * 128], NEGx[:, j * 128:(j + 1) * 128],
                            lambda pr, j=j: VA[:, j, 0:65], [tKAT, tNEG, tNEGA, tVA], False))
            yield from attn_core(kbs, OSPW[:, i, :], tOSPs[i], False, QTx, tQ)
            if i == 0:
                kprev = lambda hf, kap: KST[hf * 64:(hf + 1) * 64, 0, kap, :]
                tkp = tKST0
            else:
                kprev = lambda hf, kap: KSTW[hf * 64:(hf + 1) * 64, i - 1, kap, :]
                tkp = tKSTW[i - 1]
            kbs = [
                (kprev, SWPREV1 if c == 1 else SWPREV, lambda pr: VS[:, i, pr // 2, 0:65], [tkp, tVS[i], tC], True),
                (lambda hf, kap: KSTW[hf * 64:(hf + 1) * 64, i, kap, :], SWCUR, lambda pr: VS[:, i + 1, pr // 2, 0:65], [tKSTW[i], tVS[i + 1], tC], True),
            ]
            yield from attn_core(kbs, OWPW[:, i, :], tOWPs[i], True, QTx, tQ)

        def tail_a(i):
            par = i % 2
            OTx, tOTx = OTs[i % 2], tOTs[i % 2]

            def tro(e):
                ins = None
                for k in range(4):
                    e.transpose(out=PTR[:, par, k * 128:(k + 1) * 128], in_=OSPW[:, i, k * 128:(k + 1) * 128], identity=CB[:, 0:128])
                for k in range(4):
                    ins = e.transpose(out=PTR[:, par, (4 + k) * 128:(5 + k) * 128], in_=OWPW[:, i, k * 128:(k + 1) * 128], identity=CB[:, 0:128])
                return ins
            S.op("pe", tro, reads=[tOSPs[i], tOWPs[i], tC], writes=[tPTR[par]])
            S.op("act", lambda e: e.copy(out=OTx, in_=PTR[:, par, :].rearrange("p (k t) -> p k t", k=8)), reads=[tPTR[par]], writes=[tOTx])

        def tail_b(i, pos_bs, pos_bw):
            OTx, tOTx = OTs[i % 2], tOTs[i % 2]
            MGx, tMGx = MGs[i % 2], tMGs[i % 2]

            def br(e):
                ins = None
                for b, pos in enumerate((pos_bs, pos_bw)):
                    for half in range(2):
                        for kc in range(4):
                            ins = e.matmul(PA[:, 2 * b + half, :], lhsT=OTx[:, 4 * b + kc, :], rhs=WR[:, pos, kc * 1024 + half * 512:kc * 1024 + (half + 1) * 512],
                                           start=(kc == 0), stop=(kc == 3))
                return ins
            S.op("pe", br, reads=[tOTx, tWR[pos_bs], tWR[pos_bw]], writes=tPA)
            M1 = RL[:, 0:2, :]
            M2 = RL[:, 2:4, :]
            S.op("dve", lambda e: e.tensor_tensor(out=M1, in0=PA[:, 0:2, :], in1=SGA[:, i, :].rearrange("p (a b) -> p a b", a=2), op=ALU.mult),
                 reads=[tPA[0], tPA[1], tSGA[i]], writes=[tRL[0], tRL[1]])
            S.op("dve", lambda e: e.tensor_tensor(out=M2, in0=PA[:, 2:4, :], in1=SGB[:, i, :].rearrange("p (a b) -> p a b", a=2), op=ALU.mult),
                 reads=[tPA[2], tPA[3], tSGB[i]], writes=[tRL[2], tRL[3]])
            S.op("pool", lambda e: e.tensor_tensor(out=MGx.rearrange("p (a b) -> p a b", a=2), in0=M1, in1=M2, op=ALU.add), reads=tRL, writes=[tMGx])

        def tail_c(i):
            MGx, tMGx = MGs[i % 2], tMGs[i % 2]
            par2 = (i + 1) % 2

            def trm(e):
                ins = None
                for k in range(8):
                    ins = e.transpose(out=PTR[:, par2, k * 128:(k + 1) * 128], in_=MGx[:, k * 128:(k + 1) * 128], identity=CB[:, 0:128])
                return ins
            S.op("pe", trm, reads=[tMGx, tC], writes=[tPTR[par2]])
            S.op("act", lambda e: e.copy(out=XT[:, :, i * 128:(i + 1) * 128], in_=PTR[:, par2, :].rearrange("p (k t) -> p k t", k=8)), reads=[tPTR[par2]], writes=[tXT])

        def out_proj(NB, pos_a, pos_b):
            n = 0
            for i in range(NB):
                for half in range(2):
                    par = n % 2
                    n += 1

                    def wo(e, i=i, half=half, par=par):
                        ins = None
                        for kc in range(8):
                            pos = pos_a if kc < 4 else pos_b
                            o = (kc % 4) * 1024 + half * 512
                            ins = e.matmul(PB[:, par, :], lhsT=XT[:, kc, i * 128:(i + 1) * 128], rhs=WR[:, pos, o:o + 512], start=(kc == 0), stop=(kc == 7))
                        return ins
                    S.op("pe", wo, reads=[tXT, tWR[pos_a], tWR[pos_b]], writes=[tPB[par]])
                    S.op("dve", lambda e, i=i, half=half, par=par: e.tensor_tensor(out=X[:, i, half * 512:(half + 1) * 512], in0=PB[:, par, :],
                                                                                   in1=X[:, i, half * 512:(half + 1) * 512], op=ALU.add),
                         reads=[tPB[par], tX[i]], writes=[tX[i]])

        def final_norm_store(NB, blks, ti):
            for i in range(NB):
                tf = tF[i]
                S.op("act", lambda e, i=i: e.activation(out=SGf, in_=X[:, i, :], func=AF.Square, accum_out=SM[:, 40 + i:41 + i]), reads=[tX[i]], writes=[tSG[0], tSG[1], tf])
                S.op("dve", lambda e, i=i: e.tensor_scalar(out=SM[:, 44 + i:45 + i], in0=SM[:, 40 + i:41 + i], scalar1=1.0 / D, scalar2=EPS, op0=ALU.mult, op1=ALU.add), reads=[tf], writes=[tf])
                S.op("pool", lambda e, i=i: e.tensor_tensor(out=SM[:, 48 + i:49 + i], in0=SM[:, 44 + i:45 + i], in1=MHALF[:, 0:1], op=ALU.pow), reads=[tf, tSM], writes=[tf])
                S.op("dve", lambda e, i=i: e.scalar_tensor_tensor(out=X[:, i, :], in0=X[:, i, :], scalar=SM[:, 48 + i:49 + i], in1=GFIN[:], op0=ALU.mult, op1=ALU.mult),
                     reads=[tX[i], tf, tC], writes=[tX[i]])
                r0 = (blks[i] - 1) * 128
                S.dma("sp", lambda e, i=i, r0=r0: e.dma_start(out=out_d[r0:r0 + 128, :], in_=X[:, i, :]), reads=[tX[i]], writes=[tOUT[ti * 4 + i]])
            if ti < 8:
                for i in range(NB):
                    r1 = (blks[i] - 1) * 128 + 512
                    S.dma("pool", lambda e, i=i, r1=r1: e.dma_start(out=X[:, i, :], in_=x_d[r1:r1 + 128, :]), writes=[tX[i]])

        for ti, blks in enumerate(tiles):
            NB = len(blks)
            if ti == 0:
                S.op("pool", lambda e: e.memset(X[:, 0, :], 0.0), writes=[tX[0]])
                S.dma("pool", lambda e: e.dma_start(out=X[112:128, 0, :], in_=meta_d), writes=[tX[0]])
            elif ti == 1:
                r0 = (blks[0] - 1) * 128
                S.dma("pool", lambda e, r0=r0: e.dma_start(out=X[:, 0:4, :], in_=x_d[r0:r0 + 512, :].rearrange("(i p) d -> p i d", p=128)), writes=tX)
            c0 = blks[0] * 128
            S.dma("pool", lambda e, c0=c0, NB=NB: e.dma_start(out=CS[:, 0:NB, :], in_=cs_d[c0:c0 + NB * 128, :].rearrange("(i p) d -> p i d", p=128)), writes=[tCS])
            if ti == 0:
                convert_group(list(range(0, 11)) + ["wd0"] + list(range(11, 15)))
                load_wd(0)
            norm_to_xt(NB, 0, 0)
            if ti == 1:
                convert_group(list(range(15, NSLOT)) + ["wd1"], first_reads=[tXT])
            ffn(0, NB, tR)
            norm_to_xt(NB, 1, 0)
            for _ in project(NB, blks, range(0, 4)):
                pass
            if ti == 0:
                S.op("pool", lambda e: e.tensor_copy(out=VS[:, 0, :, 0:64], in_=VS[:, 1, :, 0:64]), reads=[tVS[1]], writes=[tVS[0]])
                rope_block(0, 0, tile0=True)
                continue
            rope_block(0, blks[0])
            gb0 = index_bisect(0, blks[0], frac=0.42)
            next(gb0)
            gp = project(NB, blks, range(4, 8))
            p_done = b_done = False
            while not (p_done and b_done):
                if not p_done:
                    try:
                        next(gp)
                    except StopIteration:
                        p_done = True
                if not b_done:
                    try:
                        next(gb0)
                    except StopIteration:
                        b_done = True
            pos_bs = use_slot(19, hold=True)
            pos_bw = use_slot(20, hold=True)
            rope_block(1, blks[1])
            for i in range(NB):
                ga = attn_main(i, blks[i])
                na_steps = blks[i] + 3
                roped = (i + 2 >= NB)
                if i + 1 < NB:
                    gb = index_bisect(i + 1, blks[i + 1], frac=0.5)
                    next(gb)
                    ca, cb, a_done, b_done = 0, 1, False, False
                    while not (a_done and b_done):
                        if b_done or (not a_done and ca * NIT <= cb * na_steps):
                            try:
                                next(ga)
                                ca += 1
                            except StopIteration:
                                a_done = True
                            if not roped and ca >= na_steps // 2:
                                rope_block(i + 2, blks[i + 2])
                                roped = True
                        else:
                            try:
                                next(gb)
                                cb += 1
                            except StopIteration:
                                b_done = True
                    if not roped:
                        rope_block(i + 2, blks[i + 2])
                else:
                    for _ in ga:
                        pass
            tail_a(0)
            for i in range(NB):
                if i + 1 < NB:
                    tail_a(i + 1)
                tail_b(i, pos_bs, pos_bw)
                if i >= 1:
                    tail_c(i - 1)
            tail_c(NB - 1)
            load_wd(1, alias=True)
            S.op("pool", lambda e: e.tensor_copy(out=VS[:, 0, :, 0:64], in_=VS[:, 4, :, 0:64]), reads=[tVS[4]], writes=[tVS[0]])
            unhold_all()
            pos_a = use_slot(21, hold=True)
            pos_b = use_slot(22, hold=True)
            out_proj(NB, pos_a, pos_b)
            unhold_all()
            norm_to_xt(NB, 2, 0)
            ffn(1, NB, tR)
            final_norm_store(NB, blks, ti)
            if ti < 8:
                load_wd(0)
            if stage == 5 and ti == 1:
                break
        S.finish("sp", tOUT)
        S.emit(st)
    return nc


def _host_layout(inp):
    f32 = np.float32
    w_in = np.asarray(inp["w_in"][0], f32)
    cols = np.arange(3780)
    qa, ka, va = cols[0:512], cols[512:576], cols[576:640]
    qi, ki, wi = cols[640:896], cols[896:960], cols[960:964]
    qs, ksw, vsw = cols[964:1476], cols[1476:1604], cols[1604:1732]
    ga, gb = cols[1732:2756], cols[2756:3780]
    perm = np.concatenate([qa, qi, ka, ka, ki, ki, qs, ksw[:64], ksw[:64], ksw[64:], ksw[64:], va, vsw, wi])
    win_p = np.zeros((D, 4096), f32)
    win_p[:, :perm.size] = w_in[:, perm]
    win_p[:, 2048:3072] = w_in[:, ga]
    win_p[:, 3072:4096] = w_in[:, gb]

    ws = np.zeros((NSLOT, 128, 4096), f32)

    def gu_slots(wg, wu, base):
        wg = np.asarray(wg, f32).reshape(8, 128, NJ, 128)
        wu = np.asarray(wu, f32).reshape(8, 128, NJ, 128)
        both = np.stack([wg, wu], 0)
        both = both.reshape(2, 8, 128, 11, 2, 128)
        arr = both.transpose(3, 2, 4, 0, 1, 5)
        ws[base:base + 11] = arr.reshape(11, 128, 4096)

    gu_slots(inp["w_ffn1_gate"][0], inp["w_ffn1_up"][0], 0)
    gu_slots(inp["w_ffn2_gate"][0], inp["w_ffn2_up"][0], 23)
    wp = win_p.reshape(8, 128, 8, 512)
    ws[11:19] = wp.transpose(2, 1, 0, 3).reshape(8, 128, 4096)
    heads = [2 * (sl % 4) + sl // 4 for sl in range(8)]
    rperm = np.concatenate([np.arange(h * 64, (h + 1) * 64) for h in heads])
    for k, name in enumerate(("w_branch_sparse", "w_branch_swa")):
        wb = np.asarray(inp[name][0], f32)[rperm]
        ws[19 + k] = wb.reshape(4, 128, 1024).transpose(1, 0, 2).reshape(128, 4096)
    wo = np.asarray(inp["w_out"][0], f32).reshape(8, 128, 1024)
    ws[21] = wo[0:4].transpose(1, 0, 2).reshape(128, 4096)
    ws[22] = wo[4:8].transpose(1, 0, 2).reshape(128, 4096)

    wd = np.stack([np.asarray(inp["w_ffn1_down"][0], f32).reshape(NJ, 128, 1024).transpose(1, 0, 2).reshape(128, NJ * 1024),
                   np.asarray(inp["w_ffn2_down"][0], f32).reshape(NJ, 128, 1024).transpose(1, 0, 2).reshape(128, NJ * 1024)], 0)

    gcol = np.concatenate([np.asarray(inp[k][0], f32).reshape(8, 128).T for k in ("norm_ffn1", "norm_mix", "norm_ffn2")], 1)
    sink = np.asarray(inp["sinks"][0], f32)[heads]

    pos = np.maximum(np.arange(NBLK * 128, dtype=np.int32) - 112, 0).astype(f32)
    inv_freq = (1.0 / (np.float32(10000.0) ** (np.arange(0, 64, 2, dtype=f32) / np.float32(64)))).astype(f32)
    ang = (pos[:, None] * inv_freq[None, :]).astype(f32)
    cs = np.concatenate([np.cos(ang).astype(f32), np.sin(ang).astype(f32)], 1)

    q = np.arange(128)[:, None]
    s = np.arange(128)[None, :]
    cst = np.zeros((128, 1152), f32)
    cst[:, 0:512] = np.tile(np.eye(128, dtype=f32), (1, 4))
    cst[:, 512:640] = np.where(s <= q, 0.0, -1e30)
    cst[:, 640:768] = np.where(s <= q, 0.0, NEGM)
    cst[:, 768:896] = np.where(s > q, 0.0, NEGM)
    cst[:, 896:1024] = np.where((s > q) & (s >= 112), 0.0, NEGM)
    p2 = np.tile((2.0 ** -(np.arange(32, dtype=np.float64) + 1)).astype(f32)[None, :], (128, 1))
    shared = {"meta": np.ascontiguousarray(np.asarray(inp["meta_tokens"], f32)), "ws": ws, "wd": np.ascontiguousarray(wd),
              "gcol": np.ascontiguousarray(gcol), "gfin": np.asarray(inp["norm_final"], f32), "sink": np.ascontiguousarray(sink),
              "cs": cs, "cst": cst, "p2": p2}
    return shared


_NC_CACHE = {}


def kernel(**inputs):
    shared = _host_layout(inputs)
    x = np.asarray(inputs["x"], np.float32)
    if "nc" not in _NC_CACHE:
        _NC_CACHE["nc"] = build_program()
    nc = _NC_CACHE["nc"]
    in_maps = []
    for b in range(8):
        m = dict(shared)
        m["x"] = np.ascontiguousarray(x[b])
        in_maps.append(m)
    res = run_bass_kernel_spmd(nc, in_maps, core_ids=list(range(8)))
    return np.stack([np.asarray(r["out"], np.float32) for r in res.results], 0)
```
